# Optimizing a Trainium2 kernel written in Bass

```python
import jax, jax.numpy as jnp
from jax import lax
import numpy as np

D_MODEL = 2048
BATCH = 4
SEQ = 2048
DEPTH = 1

D_MIX = D_MODEL
ATTN_WIDTH = D_MIX // 2
HGRN_WIDTH = D_MIX - ATTN_WIDTH
ATTN_HEAD_DIM = 128
N_ATTN_HEADS = ATTN_WIDTH // ATTN_HEAD_DIM
HGRN_HEAD_DIM = 128
N_HGRN_HEADS = HGRN_WIDTH // HGRN_HEAD_DIM
IN_COLS = 3 * ATTN_WIDTH + 4 * HGRN_WIDTH
MOBA_BLOCK = 256
MOBA_TOPK = 3
MOBA_Q_BLOCK = 64
HGRN_CHUNK = 64
N_GROUPS = 4
EXPERTS_PER_GROUP = 4
N_EXPERTS = N_GROUPS * EXPERTS_PER_GROUP
TOPK_IN_GROUP = 2
D_EXPERT = D_MODEL // 2
RMS_EPS = 1e-6

kernel_name = "hymba_moba_hgrn2_hmoe_block"


def rms_norm(x, g):
    xf = x.astype(jnp.float32)
    y = xf * lax.rsqrt(jnp.mean(xf * xf, axis=-1, keepdims=True) + RMS_EPS)
    return (y * g.astype(jnp.float32)).astype(x.dtype)


def alibi_slopes(n_heads):
    return jnp.asarray(2.0 ** (-8.0 * np.arange(1, n_heads + 1) / n_heads), dtype=jnp.float32)


def moba_attention(q, k, v):
    B, H, T, Dh = q.shape
    Tp = -(-T // MOBA_BLOCK) * MOBA_BLOCK
    if Tp != T:
        pad = ((0, 0), (0, 0), (0, Tp - T), (0, 0))
        q, k, v = jnp.pad(q, pad), jnp.pad(k, pad), jnp.pad(v, pad)
    nb = Tp // MOBA_BLOCK
    k_top = min(MOBA_TOPK, nb)
    scale = Dh ** -0.5
    slopes = alibi_slopes(H)
    kb = k.reshape(B, H, nb, MOBA_BLOCK, Dh)
    vb = v.reshape(B, H, nb, MOBA_BLOCK, Dh)
    k_mean = jnp.mean(kb.astype(jnp.float32), axis=3)
    gate = jnp.einsum('bhtd,bhnd->bhtn', q.astype(jnp.float32), k_mean)
    q_blk = jnp.arange(Tp) // MOBA_BLOCK
    past = jnp.arange(nb)[None, :] < q_blk[:, None]
    gate = jnp.where(past[None, None], gate, -jnp.inf)
    _, sel = lax.top_k(gate, k_top)
    sel_valid = sel < q_blk[None, None, :, None]

    n_qb = Tp // MOBA_Q_BLOCK

    def to_blocks(a):
        a = a.reshape(B, H, n_qb, MOBA_Q_BLOCK, *a.shape[3:])
        return jnp.moveaxis(a, 2, 0)

    gather_blocks = jax.vmap(jax.vmap(lambda blocks, idx: blocks[idx]))

    def step(args):
        qc, selc, validc, ci = args
        t_pos = ci * MOBA_Q_BLOCK + jnp.arange(MOBA_Q_BLOCK)
        own = (ci * MOBA_Q_BLOCK) // MOBA_BLOCK
        k_g = gather_blocks(kb, selc)
        v_g = gather_blocks(vb, selc)
        s_sel = jnp.einsum('bhqd,bhqkcd->bhqkc', qc, k_g,
                           preferred_element_type=jnp.float32) * scale
        key_pos = selc[..., None] * MOBA_BLOCK + jnp.arange(MOBA_BLOCK)
        dist_sel = (t_pos[None, None, :, None, None] - key_pos).astype(jnp.float32)
        s_sel = s_sel - slopes[None, :, None, None, None] * dist_sel
        s_sel = jnp.where(validc[..., None], s_sel, -jnp.inf)
        k_own = lax.dynamic_slice_in_dim(k, own * MOBA_BLOCK, MOBA_BLOCK, axis=2)
        v_own = lax.dynamic_slice_in_dim(v, own * MOBA_BLOCK, MOBA_BLOCK, axis=2)
        s_own = jnp.einsum('bhqd,bhcd->bhqc', qc, k_own,
                           preferred_element_type=jnp.float32) * scale
        dist_own = t_pos[:, None] - (own * MOBA_BLOCK + jnp.arange(MOBA_BLOCK))[None, :]
        s_own = s_own - slopes[:, None, None] * dist_own.astype(jnp.float32)
        s_own = jnp.where((dist_own >= 0)[None, None], s_own, -jnp.inf)
        n_sel = k_top * MOBA_BLOCK
        s = jnp.concatenate([s_sel.reshape(B, H, MOBA_Q_BLOCK, n_sel), s_own], axis=-1)
        p = jax.nn.softmax(s, axis=-1).astype(v.dtype)
        p_sel = p[..., :n_sel].reshape(B, H, MOBA_Q_BLOCK, k_top, MOBA_BLOCK)
        p_own = p[..., n_sel:]
        return (jnp.einsum('bhqkc,bhqkcd->bhqd', p_sel, v_g)
                + jnp.einsum('bhqc,bhcd->bhqd', p_own, v_own))

    out = lax.map(step, (to_blocks(q), to_blocks(sel), to_blocks(sel_valid),
                         jnp.arange(n_qb)))
    out = jnp.moveaxis(out, 0, 2).reshape(B, H, Tp, Dh)
    return out[:, :, :T]


def hgrn2(q, f_logit, i, g, lb, out_norm_g):
    B, T, H, Dk = q.shape
    Dv = i.shape[-1]
    dt = q.dtype
    qf = jax.nn.silu(q.astype(jnp.float32))
    f = lb.astype(jnp.float32) + (1.0 - lb.astype(jnp.float32)) * jax.nn.sigmoid(f_logit.astype(jnp.float32))
    kf = 1.0 - f
    log_f = jnp.log(f)
    vf = i.astype(jnp.float32)
    C = HGRN_CHUNK
    n = T // C

    def to_chunks(a):
        return a.reshape(B, n, C, H, a.shape[-1]).transpose(1, 0, 3, 2, 4)

    causal = jnp.tril(jnp.ones((C, C), dtype=bool))

    def chunk_step(S, inp):
        qc, kc, vc, lfc = inp
        b = jnp.cumsum(lfc, axis=2)
        inter = jnp.einsum('bhtk,bhkv->bhtv', qc * jnp.exp(b), S)
        diff = b[:, :, :, None, :] - b[:, :, None, :, :]
        decay = jnp.exp(jnp.where(causal[None, None, :, :, None], diff, -jnp.inf))
        A = jnp.einsum('bhtk,bhsk,bhtsk->bhts', qc, kc, decay)
        intra = jnp.einsum('bhts,bhsv->bhtv', A, vc)
        b_last = b[:, :, -1:, :]
        S_new = (jnp.exp(b_last[:, :, 0, :])[..., None] * S
                 + jnp.einsum('bhsk,bhsv->bhkv', kc * jnp.exp(b_last - b), vc))
        return S_new, inter + intra

    S0 = jnp.zeros((B, H, Dk, Dv), jnp.float32)
    _, o = lax.scan(chunk_step, S0, (to_chunks(qf), to_chunks(kf), to_chunks(vf), to_chunks(log_f)))
    o = o.transpose(1, 0, 3, 2, 4).reshape(B, T, H, Dv)
    o = rms_norm(o, out_norm_g) * jax.nn.silu(g.astype(jnp.float32))
    return o.reshape(B, T, H * Dv).astype(dt)


def hier_moe(h, w_grp, b_grp, w_er, b_er, w_gate, w_up, w_down):
    B, T, D = h.shape
    hf = h.reshape(B * T, D)
    grp_prob = jax.nn.softmax((hf @ w_grp).astype(jnp.float32) + b_grp.astype(jnp.float32), axis=-1)
    grp_w, grp_idx = lax.top_k(grp_prob, 1)
    exp_logits = (jnp.einsum('nd,gde->nge', hf, w_er).astype(jnp.float32)
                  + b_er.astype(jnp.float32)[None])
    exp_logits = jnp.take_along_axis(exp_logits, grp_idx[:, :, None], axis=1)[:, 0]
    exp_prob = jax.nn.softmax(exp_logits, axis=-1)
    top_w, top_idx = lax.top_k(exp_prob, TOPK_IN_GROUP)
    top_w = top_w / jnp.sum(top_w, axis=-1, keepdims=True) * grp_w
    global_idx = grp_idx * EXPERTS_PER_GROUP + top_idx
    combine = jnp.sum(jax.nn.one_hot(global_idx, N_EXPERTS, dtype=jnp.float32)
                      * top_w[..., None], axis=1).astype(h.dtype)
    out = jnp.zeros_like(hf)
    for gi in range(N_GROUPS):
        sl = slice(gi * EXPERTS_PER_GROUP, (gi + 1) * EXPERTS_PER_GROUP)
        a = jnp.einsum('nd,edf->nef', hf, w_gate[sl])
        u = jnp.einsum('nd,edf->nef', hf, w_up[sl])
        hid = jax.nn.silu(a) * u * combine[:, sl, None]
        out = out + jnp.einsum('nef,efd->nd', hid, w_down[sl])
    return out.reshape(B, T, D)


def setup_inputs(seed: int = 0) -> dict:
    key = jax.random.key(seed)
    ks = jax.random.split(key, 16)
    f32 = jnp.float32

    def nrm(k, shape, scale):
        return jax.random.normal(k, shape, f32) * scale

    return {
        "x": nrm(ks[0], (BATCH, SEQ, D_MODEL), 1.0),
        "norm_mix_g": 1.0 + nrm(ks[1], (DEPTH, D_MODEL), 0.02),
        "w_in": nrm(ks[2], (DEPTH, D_MODEL, IN_COLS), D_MODEL ** -0.5),
        "hgrn_lb_logits": nrm(ks[3], (DEPTH + 1, HGRN_WIDTH), 0.5),
        "hgrn_out_norm_g": 1.0 + nrm(ks[4], (DEPTH, HGRN_HEAD_DIM), 0.02),
        "w_out": nrm(ks[5], (DEPTH, D_MIX, D_MODEL), D_MIX ** -0.5),
        "norm_ffn_g": 1.0 + nrm(ks[6], (DEPTH, D_MODEL), 0.02),
        "w_group_router": nrm(ks[7], (DEPTH, D_MODEL, N_GROUPS), D_MODEL ** -0.5),
        "b_group_router": nrm(ks[8], (DEPTH, N_GROUPS), 0.01),
        "w_expert_router": nrm(ks[9], (DEPTH, N_GROUPS, D_MODEL, EXPERTS_PER_GROUP), D_MODEL ** -0.5),
        "b_expert_router": nrm(ks[10], (DEPTH, N_GROUPS, EXPERTS_PER_GROUP), 0.01),
        "w_gate": nrm(ks[11], (DEPTH, N_EXPERTS, D_MODEL, D_EXPERT), D_MODEL ** -0.5),
        "w_up": nrm(ks[12], (DEPTH, N_EXPERTS, D_MODEL, D_EXPERT), D_MODEL ** -0.5),
        "w_down": nrm(ks[13], (DEPTH, N_EXPERTS, D_EXPERT, D_MODEL), D_EXPERT ** -0.5),
        "final_norm_g": 1.0 + nrm(ks[14], (D_MODEL,), 0.02),
    }


def reference(x, norm_mix_g, w_in, hgrn_lb_logits, hgrn_out_norm_g, w_out, norm_ffn_g,
              w_group_router, b_group_router, w_expert_router, b_expert_router,
              w_gate, w_up, w_down, final_norm_g):
    B, T, _ = x.shape
    lb_all = jnp.cumsum(jax.nn.softmax(hgrn_lb_logits.astype(jnp.float32), axis=0), axis=0)[:DEPTH]
    for l in range(DEPTH):
        h = rms_norm(x, norm_mix_g[l])
        proj = h @ w_in[l]
        o0 = 0
        q_a = proj[..., o0:o0 + ATTN_WIDTH]; o0 += ATTN_WIDTH
        k_a = proj[..., o0:o0 + ATTN_WIDTH]; o0 += ATTN_WIDTH
        v_a = proj[..., o0:o0 + ATTN_WIDTH]; o0 += ATTN_WIDTH
        q_r = proj[..., o0:o0 + HGRN_WIDTH]; o0 += HGRN_WIDTH
        f_r = proj[..., o0:o0 + HGRN_WIDTH]; o0 += HGRN_WIDTH
        i_r = proj[..., o0:o0 + HGRN_WIDTH]; o0 += HGRN_WIDTH
        g_r = proj[..., o0:o0 + HGRN_WIDTH]

        def heads(a):
            return a.reshape(B, T, N_ATTN_HEADS, ATTN_HEAD_DIM).transpose(0, 2, 1, 3)

        o_attn = moba_attention(heads(q_a), heads(k_a), heads(v_a))
        o_attn = o_attn.transpose(0, 2, 1, 3).reshape(B, T, ATTN_WIDTH)

        def rheads(a):
            return a.reshape(B, T, N_HGRN_HEADS, HGRN_HEAD_DIM)

        o_rec = hgrn2(rheads(q_r), rheads(f_r), rheads(i_r), rheads(g_r),
                      lb_all[l].reshape(N_HGRN_HEADS, HGRN_HEAD_DIM), hgrn_out_norm_g[l])
        x = x + jnp.concatenate([o_attn, o_rec], axis=-1) @ w_out[l]

        h2 = rms_norm(x, norm_ffn_g[l])
        x = x + hier_moe(h2, w_group_router[l], b_group_router[l], w_expert_router[l],
                         b_expert_router[l], w_gate[l], w_up[l], w_down[l])
    return rms_norm(x, final_norm_g)
```

```python
import contextlib
import numpy as np
import ml_dtypes
import concourse.bass as bass
import concourse.mybir as mybir
from concourse.bass_utils import run_bass_kernel_spmd

F32 = mybir.dt.float32
BF16 = mybir.dt.bfloat16
AF = mybir.ActivationFunctionType
ALU = mybir.AluOpType
AX = mybir.AxisListType

ENGS = ("pe", "act", "dve", "pool", "sp")


class Op:
    __slots__ = ("eng", "fn", "reads", "writes", "dma", "key", "sig", "sigval",
                 "idx", "dmaval", "waits", "epoch")

    def __init__(self, eng, fn, reads, writes, dma, key, epoch):
        self.eng = eng
        self.fn = fn
        self.reads = reads
        self.writes = writes
        self.dma = dma
        self.key = key
        self.sig = False
        self.sigval = 0
        self.dmaval = 0
        self.waits = []
        self.epoch = epoch


class Prog:
    def __init__(self, nc, same_engine_sync=("act", "dve", "pool")):
        self.nc = nc
        self.ops = []
        self.same = set(same_engine_sync)
        self.state = {}
        self.children = {}
        self.dma_count = {}
        self.epoch = 0
        self.nsig = {e: 0 for e in ENGS}

    def _related(self, tok):
        out = []
        for i in range(1, len(tok) + 1):
            p = tok[:i]
            if p in self.state:
                out.append(p)
        for t in self.children.get(tok, ()):
            out.append(t)
        return out

    def _touch(self, tok):
        if tok not in self.state:
            self.state[tok] = [None, []]
            for i in range(1, len(tok)):
                self.children.setdefault(tok[:i], set()).add(tok)
        return self.state[tok]

    def add(self, eng, fn, reads=(), writes=(), dma=False, key=None):
        op = Op(eng, fn, [tuple(r) for r in reads], [tuple(w) for w in writes], dma, key, self.epoch)
        op.idx = len(self.ops)
        deps = {}
        for r in op.reads:
            for t in self._related(r):
                w = self.state[t][0]
                if w is not None:
                    deps[id(w)] = w
        for wtok in op.writes:
            for t in self._related(wtok):
                st = self.state[t]
                if st[0] is not None:
                    deps[id(st[0])] = st[0]
                for rd in st[1]:
                    deps[id(rd)] = rd
        for d in deps.values():
            self._dep(op, d)
        for r in op.reads:
            rl = self._touch(r)[1]
            if not dma:
                rl[:] = [o for o in rl if o.dma or o.eng != eng]
            rl.append(op)
        for wtok in op.writes:
            st = self._touch(wtok)
            st[0] = op
            st[1] = []
            for t in list(self.children.get(wtok, ())):
                self.state[t] = [op, []]
        if dma:
            self.dma_count[key] = self.dma_count.get(key, 0) + 16
            op.dmaval = self.dma_count[key]
        self.ops.append(op)
        return op

    def _dep(self, op, d):
        if d.dma:
            op.waits.append(("dma", d.key, self.dma_count[d.key]))
        else:
            if d.eng == op.eng and not op.dma and d.eng not in self.same:
                return
            if not d.sig:
                d.sig = True
                self.nsig[d.eng] += 1
            op.waits.append(("eng", d))

    def barrier(self):
        last_nd = {}
        for o in reversed(self.ops):
            if o.epoch != self.epoch:
                break
            if not o.dma and o.eng not in last_nd:
                last_nd[o.eng] = o
            if len(last_nd) == len(ENGS):
                break
        dkeys = dict(self.dma_count)
        for eng in ENGS:
            def nopfn(e):
                return e.nop()
            op = Op(eng, nopfn, [], [], False, None, self.epoch)
            op.idx = len(self.ops)
            for o in last_nd.values():
                if o.eng == eng and eng not in self.same:
                    continue
                if not o.sig:
                    o.sig = True
                op.waits.append(("eng", o))
            for k, v in dkeys.items():
                op.waits.append(("dma", k, v))
            self.ops.append(op)
        self.state = {}
        self.children = {}
        self.epoch += 1
        self.nsig = {e: 0 for e in ENGS}

    def maybe_barrier(self, limit):
        if max(self.nsig.values()) > limit:
            self.barrier()

    def emit(self):
        nc = self.nc
        cnt = {}
        for op in self.ops:
            if op.sig and not op.dma:
                k = (op.eng, op.epoch)
                cnt[k] = cnt.get(k, 0) + 1
                op.sigval = cnt[k]
        self.sigcounts = cnt
        keys = sorted(self.dma_count.keys(), key=str)
        with contextlib.ExitStack() as st:
            esem = {}
            for ep in range(self.epoch + 1):
                for e in ENGS:
                    if (e, ep) in cnt:
                        esem[(e, ep)] = st.enter_context(nc.semaphore("s_%s_%d" % (e, ep)))
            dsem = {k: st.enter_context(nc.semaphore("d_%d" % i)) for i, k in enumerate(keys)}
            self.nsems = len(esem) + len(dsem)
            block = st.enter_context(nc.Block())

            def make(engname):
                def body(e):
                    waited = {}
                    for op in self.ops:
                        if op.eng != engname:
                            continue
                        for w in op.waits:
                            if w[0] == "dma":
                                sem, val, kk = dsem[w[1]], w[2], ("d", w[1])
                            else:
                                kk = (w[1].eng, w[1].epoch)
                                sem, val = esem[kk], w[1].sigval
                            if waited.get(kk, 0) >= val:
                                continue
                            waited[kk] = val
                            e.wait_ge(sem, val)
                        ins = op.fn(e)
                        if op.dma:
                            ins.then_inc(dsem[op.key], 16)
                        elif op.sig:
                            ins.then_inc(esem[(op.eng, op.epoch)], 1)
                    if engname == "sp":
                        for k in keys:
                            e.wait_ge(dsem[k], self.dma_count[k])
                return body

            block.tensor(make("pe"))
            block.scalar(make("act"))
            block.vector(make("dve"))
            block.gpsimd(make("pool"))
            block.sync(make("sp"))


D = 2048
TT = 2048
TO = 1024
NCH = 16
NH = 8
DH = 128
INC = 7168
NE = 16
DE = 1024
EPS = 1e-6
NEG = -30000.0
QA0, KA0, VA0, QR0, FR0, IR0, GR0 = 0, 1024, 2048, 3072, 4096, 5120, 6144


class _Stop(Exception):
    pass


def build_nc(debug=False, upto=9, nheads=NH):
    try:
        return _build_nc(debug, upto, nheads)
    except _Stop as s:
        return s.args[0]


def _build_nc(debug, upto, nheads):
    nc = bass.Bass("TRN2", target_bir_lowering=False)

    def din(name, shape, dt=F32):
        return nc.dram_tensor(name, list(shape), dt, kind="ExternalInput").ap()

    xc = din("xc", [TT, D])
    w_in = din("w_in", [D, INC])
    w_out = din("w_out", [D, D])
    w_gate = din("w_gate", [NE, D, DE])
    w_up = din("w_up", [NE, D, DE])
    w_down = din("w_down", [NE, DE, D])
    wr = din("wr", [D, 20])
    br = din("br", [1, 20])
    g_mix = din("g_mix", [1, D])
    g_ffn = din("g_ffn", [1, D])
    g_fin = din("g_fin", [1, D])
    lbl = din("lbl", [128, 16])
    ong_d = din("ong", [128, 1])
    c_ident = din("c_ident", [128, 128], BF16)
    c_gmask = din("c_gmask", [128, 3 * 64])
    c_en = din("c_en", [128, 8 * 128], BF16)
    c_cb = din("c_cb", [128, 4 * 512], BF16)
    c_bcol = din("c_bcol", [128, 8 * 2 * 16])
    c_arow = din("c_arow", [8, 2, 1024], BF16)
    c_m01 = din("c_m01", [64, 64])
    y_out = nc.dram_tensor("y", [TO, D], F32, kind="ExternalOutput").ap()
    dbg = {}
    if debug:
        dbg["oT"] = nc.dram_tensor("dbg_oT", [128, 16 * TO], BF16, kind="ExternalOutput").ap()
        dbg["x1"] = nc.dram_tensor("dbg_x1", [128, 8 * D], F32, kind="ExternalOutput").ap()
        dbg["comb"] = nc.dram_tensor("dbg_comb", [128, 8 * 16], F32, kind="ExternalOutput").ap()
        dbg["hT"] = nc.dram_tensor("dbg_hT", [128, 16 * TT], BF16, kind="ExternalOutput").ap()

    P = Prog(nc)

    def I(eng, name, *args, reads=(), writes=(), dma=False, key=None, **kw):
        def fn(e, name=name, args=args, kw=kw):
            return getattr(e, name)(*args, **kw)
        return P.add(eng, fn, reads, writes, dma, key)

    def DMA(eng, out, in_, reads=(), writes=(), key=None):
        return I(eng, "dma_start", out=out, in_=in_, reads=reads, writes=writes, dma=True, key=key)

    isq = float(DH) ** -0.5

    with contextlib.ExitStack() as top:
        T_ = top.enter_context
        psA = [T_(nc.psum_tensor("psA%d" % i, [128, 512], F32)) for i in range(2)]
        psS = [T_(nc.psum_tensor("psS%d" % i, [128, 512], F32)) for i in range(2)]
        psO = T_(nc.psum_tensor("psO", [128, 512], F32))
        psR = T_(nc.psum_tensor("psR", [128, 512], F32))
        psT = T_(nc.psum_tensor("psT", [128, 8, 128], BF16))
        psH = T_(nc.psum_tensor("psH", [128, 512], F32))
        oT = T_(nc.sbuf_tensor("oT", [128, 16, TO], BF16))
        ident = T_(nc.sbuf_tensor("ident", [128, 128], BF16))
        ones_b = T_(nc.sbuf_tensor("ones_b", [128, 128], BF16))
        epsc = T_(nc.sbuf_tensor("epsc", [128, 1], F32))
        ss = T_(nc.sbuf_tensor("ss", [128, 32], F32))
        rstd = T_(nc.sbuf_tensor("rstd", [128, 32], F32))

        DMA("sp", ident[:], c_ident, writes=[("ident",)], key="c0")
        I("dve", "memset", ones_b[:], 1.0, writes=[("ones_b",)])
        I("dve", "memset", epsc[:], EPS, writes=[("epsc",)])

        acc_ctr = [0]

        def next_psA():
            i = acc_ctr[0] % 2
            acc_ctr[0] += 1
            return i

        cp_ctr = [0]

        def evac_copy(out, in_, reads, writes):
            i = cp_ctr[0] % 2
            cp_ctr[0] += 1
            if i == 0:
                I("act", "copy", out=out, in_=in_, reads=reads, writes=writes)
            else:
                I("dve", "tensor_copy", out=out, in_=in_, reads=reads, writes=writes)

        def rms_tile(gB, src_ap, srctoks, idx, hn_ap, hn_tok, junk_ap, junk_tok):
            I("act", "activation", out=junk_ap, in_=src_ap, func=AF.Square, accum_out=ss[:, idx:idx + 1],
              reads=srctoks, writes=[junk_tok, ("ss", idx)])
            I("act", "activation", out=rstd[:, idx:idx + 1], in_=ss[:, idx:idx + 1], func=AF.Sqrt, bias=epsc[:], scale=1.0 / D,
              reads=[("ss", idx), ("epsc",)], writes=[("rstd", idx)])
            I("dve", "reciprocal", out=rstd[:, idx:idx + 1], in_=rstd[:, idx:idx + 1], reads=[("rstd", idx)], writes=[("rstd", idx)])
            I("dve", "scalar_tensor_tensor", out=hn_ap, in0=src_ap, scalar=rstd[:, idx:idx + 1], in1=gB[:], op0=ALU.mult, op1=ALU.mult,
              reads=list(srctoks) + [("rstd", idx), ("gB",)], writes=[hn_tok])

        with contextlib.ExitStack() as sHT:
            hT = sHT.enter_context(nc.sbuf_tensor("hT", [128, NCH, TT], BF16))
            with contextlib.ExitStack() as s1:
                S_ = s1.enter_context
                gB = S_(nc.sbuf_tensor("gB1", [128, D], F32))
                xt = [S_(nc.sbuf_tensor("xt%d" % i, [128, D], F32)) for i in range(4)]
                hn = [S_(nc.sbuf_tensor("hn%d" % i, [128, D], BF16)) for i in range(4)]
                DMA("sp", gB[:], g_mix.broadcast_to([128, D]), writes=[("gB",)], key="gB")
                for i in range(16):
                    b_ = i % 4
                    DMA("sp", xt[b_][:], xc[i * 128:(i + 1) * 128, :], writes=[("xt", b_)], key=("xt", b_))
                    rms_tile(gB, xt[b_][:], [("xt", b_)], i, hn[b_][:], ("hn", b_), hn[b_][:], ("hn", b_))
                    for half in range(2):
                        for c in range(8):
                            cc = half * 8 + c
                            I("pe", "transpose", out=psT[:, c, :], in_=hn[b_][:, cc * 128:(cc + 1) * 128], identity=ident[:],
                              reads=[("hn", b_), ("ident",)], writes=[("psT", c)])
                        evac_copy(hT[:, half * 8:(half + 1) * 8, i * 128:(i + 1) * 128], psT[:], [("psT",)], [("hT", i)])
                if upto == 1:
                    DMA("sp", dbg["hT"], hT[:].rearrange("p a b -> p (a b)"), reads=[("hT",)], key="dbg0")
                    P.emit()
                    raise _Stop(nc)
                P.barrier()

            with contextlib.ExitStack() as s2:
                S_ = s2.enter_context
                NWP = 6
                Wp = [S_(nc.sbuf_tensor("Wp%d" % i, [128, NCH, 128], BF16)) for i in range(NWP)]
                QT = S_(nc.sbuf_tensor("QT", [128, TO], BF16))
                KT = S_(nc.sbuf_tensor("KT", [128, TT], BF16))
                V = S_(nc.sbuf_tensor("V", [128, 16, 128], BF16))
                MRT = S_(nc.sbuf_tensor("MRT", [128, TO], BF16))
                PT = [S_(nc.sbuf_tensor("PT%d" % i, [128, 512], BF16)) for i in range(3)]
                rinv = S_(nc.sbuf_tensor("rinv", [128, 512], F32))
                km = S_(nc.sbuf_tensor("km", [128, 8], F32))
                kmb = S_(nc.sbuf_tensor("kmb", [128, 8], BF16))
                gm = S_(nc.sbuf_tensor("gm", [128, 64], F32))
                rank = S_(nc.sbuf_tensor("rank", [128, 64], F32))
                MRq = S_(nc.sbuf_tensor("MRq", [128, 64], BF16))
                gmask = S_(nc.sbuf_tensor("gmask", [128, 3, 64], F32))
                En = S_(nc.sbuf_tensor("En", [128, 8, 128], BF16))
                CB = S_(nc.sbuf_tensor("CB", [128, 4, 512], BF16))
                bcol = S_(nc.sbuf_tensor("bcol", [128, 8, 2, 16], F32))
                LF = S_(nc.sbuf_tensor("LF", [128, TT], F32))
                KK = S_(nc.sbuf_tensor("KK", [128, TT], BF16))
                tmpB = S_(nc.sbuf_tensor("tmpB", [128, TT], BF16))
                E1 = S_(nc.sbuf_tensor("E1", [128, TO], BF16))
                Vi = S_(nc.sbuf_tensor("Vi", [64, 32, 128], BF16))
                kdT = S_(nc.sbuf_tensor("kdT", [64, 32, 128], BF16))
                sqt = [S_(nc.sbuf_tensor("sqt%d" % i, [128, 512], BF16)) for i in range(2)]
                qe = S_(nc.sbuf_tensor("qe", [128, TO], BF16))
                sg = S_(nc.sbuf_tensor("sg", [128, TO], BF16))
                OT = S_(nc.sbuf_tensor("OT", [128, TO], F32))
                rsn = S_(nc.sbuf_tensor("rsn", [128, 512], F32))
                Smb = S_(nc.sbuf_tensor("Smb", [128, 16, 128], BF16))
                Rst = S_(nc.sbuf_tensor("Rst", [128, 128], F32))
                T1 = [S_(nc.sbuf_tensor("T1_%d" % i, [128, 128], F32)) for i in range(2)]
                ATs = S_(nc.sbuf_tensor("ATs", [64, 8, 64], BF16))
                m01 = S_(nc.sbuf_tensor("m01", [64, 64], F32))
                onesf = S_(nc.sbuf_tensor("onesf", [128, 512], F32))
                Bmc = S_(nc.sbuf_tensor("Bmc", [128, 32], F32))
                gcol = S_(nc.sbuf_tensor("gcol", [128, 32], F32))
                lbt = S_(nc.sbuf_tensor("lbt", [128, 16], F32))
                lbc = S_(nc.sbuf_tensor("lbc", [128, 8], F32))
                oml = S_(nc.sbuf_tensor("oml", [128, 8], F32))
                ong = S_(nc.sbuf_tensor("ong_sb", [128, 1], F32))
                iT = tmpB
                E2 = tmpB
                sqo = E1

                DMA("sp", gmask[:], c_gmask.rearrange("p (a b) -> p a b", a=3), writes=[("gmask",)], key="c1")
                DMA("sp", En[:], c_en.rearrange("p (a b) -> p a b", a=8), writes=[("En",)], key="c2")
                DMA("sp", CB[:], c_cb.rearrange("p (a b) -> p a b", a=4), writes=[("CB",)], key="c3")
                DMA("sp", bcol[:], c_bcol.rearrange("p (a b c) -> p a b c", a=8, b=2), writes=[("bcol",)], key="c4")
                DMA("sp", m01[:], c_m01, writes=[("m01",)], key="c5")
                DMA("sp", lbt[:], lbl, writes=[("lbt",)], key="c6")
                DMA("sp", ong[:], ong_d, writes=[("ong",)], key="c7")
                I("dve", "memset", onesf[:], 1.0, writes=[("onesf",)])
                I("dve", "memset", MRT[:], 0.0, writes=[("MRT",)])
                I("dve", "tensor_tensor", out=lbc[:], in0=lbt[:, 0:8], in1=lbt[:, 8:16], op=ALU.subtract, reads=[("lbt",)], writes=[("lbc",)])
                I("act", "activation", out=lbc[:], in_=lbc[:], func=AF.Sigmoid, reads=[("lbc",)], writes=[("lbc",)])
                I("dve", "tensor_scalar", out=oml[:], in0=lbc[:], scalar1=-1.0, scalar2=1.0, op0=ALU.mult, op1=ALU.add,
                  reads=[("lbc",)], writes=[("oml",)])

                wslot = [0]

                def load_w(col0):
                    s = wslot[0] % NWP
                    wslot[0] += 1
                    DMA("pool", Wp[s][:], w_in[:, col0:col0 + 128].rearrange("(c p) n -> p c n", p=128), writes=[("Wp", s)], key=("Wp", s))
                    return s

                def proj_fm(s, tg, evac):
                    pi = next_psA()
                    for c in range(NCH):
                        I("pe", "matmul", psA[pi][:], lhsT=Wp[s][:, c, :], rhs=hT[:, c, tg * 512:(tg + 1) * 512],
                          start=(c == 0), stop=(c == NCH - 1),
                          reads=[("Wp", s)] + [("hT", tg * 4 + k) for k in range(4)], writes=[("psA", pi)])
                    evac(pi)

                bg = []

                def run_bg(n):
                    for _ in range(n):
                        if bg:
                            bg.pop(0)()

                import os as _os
                _sub = int(_os.environ.get("KSUB", "0"))

                _subh = int(_os.environ.get("KSUBH", "0"))
                cur_h = [0]

                def ck(n):
                    if _sub == n and cur_h[0] == _subh:
                        for _k in range(int(_os.environ.get("KDUMPE", "0"))):
                            I("pe", "matmul", psH[:, 2, :], lhsT=kdT[:, 6, :], rhs=Vi[:, 6, :], start=True, stop=True,
                              reads=[("kdT", 0), ("Vi", 0)], writes=[("psH", 2)])
                        for _k in range(int(_os.environ.get("KDUM", "0"))):
                            I("dve", "memset", rank[:], 0.0, writes=[("rank",)])
                        DMA("sp", dbg["oT"], oT[:].rearrange("p a b -> p (a b)"), reads=[("oT",)], key="dbg1")
                        P.emit()
                        raise _Stop(nc)

                for h in range(nheads):
                    cur_h[0] = h
                    if h == 0:
                        pre = [load_w(IR0), load_w(FR0), load_w(QR0), load_w(GR0)]
                    sI, sF, sQ, sG = pre
                    for tg in range(4):
                        def ev_i(pi, tg=tg):
                            evac_copy(iT[:, tg * 512:(tg + 1) * 512], psA[pi][:], [("psA", pi)], [("tmpB", tg)])
                        proj_fm(sI, tg, ev_i)
                    for grp in range(4):
                        for k in range(8):
                            c = grp * 8 + k
                            I("pe", "transpose", out=psT[0:64, k, :], in_=iT[:, c * 64:(c + 1) * 64], identity=ident[:],
                              reads=[("tmpB", c // 8), ("ident",)], writes=[("psT", k)])
                        evac_copy(Vi[:, grp * 8:(grp + 1) * 8, :], psT[0:64, :, :], [("psT",)], [("Vi", grp)])
                    ck(1)
                    for tg in range(4):
                        def ev_f(pi, tg=tg):
                            I("act", "activation", out=LF[:, tg * 512:(tg + 1) * 512], in_=psA[pi][:], func=AF.Sigmoid,
                              reads=[("psA", pi)], writes=[("LF", tg)])
                            I("act", "activation", out=KK[:, tg * 512:(tg + 1) * 512], in_=psA[pi][:], func=AF.Sigmoid, scale=-1.0,
                              reads=[("psA", pi)], writes=[("KK", tg)])
                        proj_fm(sF, tg, ev_f)
                    LF3 = LF[:].rearrange("p (c s) -> p c s", s=64)
                    chain = []
                    chain.append(lambda h=h: I("dve", "tensor_scalar", out=LF[:], in0=LF[:], scalar1=oml[:, h:h + 1], scalar2=lbc[:, h:h + 1],
                                               op0=ALU.mult, op1=ALU.add, reads=[("LF",), ("oml",), ("lbc",)], writes=[("LF",)]))
                    chain.append(lambda: I("act", "activation", out=LF[:], in_=LF[:], func=AF.Ln, reads=[("LF",)], writes=[("LF",)]))
                    for k in range(4):
                        init = 0.0 if k == 0 else LF[:, k * 512 - 1:k * 512]
                        chain.append(lambda k=k, init=init: I("dve", "tensor_tensor_scan", out=LF[:, k * 512:(k + 1) * 512], data0=onesf[:],
                                                              data1=LF[:, k * 512:(k + 1) * 512], initial=init, op0=ALU.mult, op1=ALU.add,
                                                              reads=[("LF",), ("onesf",)], writes=[("LF",)]))

                    def _bm():
                        I("dve", "tensor_copy", out=Bmc[:], in_=LF3[:, :, 31], reads=[("LF",)], writes=[("Bmc",)])
                        I("dve", "tensor_tensor", out=gcol[:, 0:31], in0=Bmc[:, 1:32], in1=Bmc[:, 0:31], op=ALU.subtract,
                          reads=[("Bmc",)], writes=[("gcol",)])
                    chain.append(_bm)
                    chain.append(lambda: I("dve", "tensor_tensor", out=LF3, in0=LF3, in1=Bmc[:].unsqueeze(2).broadcast_to([128, 32, 64]),
                                           op=ALU.subtract, reads=[("LF",), ("Bmc",)], writes=[("LF",)]))
                    chain.append(lambda: I("act", "activation", out=gcol[:, 0:31], in_=gcol[:, 0:31], func=AF.Exp, reads=[("gcol",)], writes=[("gcol",)]))
                    chain.append(lambda: I("act", "activation", out=E2[:], in_=LF[:], func=AF.Exp, scale=-1.0, reads=[("LF",)], writes=[("tmpB",)]))
                    chain.append(lambda: I("act", "activation", out=E1[:], in_=LF[:, TO:TT], func=AF.Exp, reads=[("LF",)], writes=[("E1",)]))
                    chain.append(lambda h=h: I("dve", "scalar_tensor_tensor", out=KK[:], in0=KK[:], scalar=oml[:, h:h + 1], in1=E2[:],
                                               op0=ALU.mult, op1=ALU.mult, reads=[("KK",), ("tmpB",), ("oml",)], writes=[("KK",)]))

                    def after_group():
                        if chain:
                            chain.pop(0)()

                    for tgo in range(2):
                        def ev_q(pi, tgo=tgo):
                            I("act", "activation", out=sqt[tgo][:], in_=psA[pi][:], func=AF.Silu, reads=[("psA", pi)], writes=[("sqt", tgo)])
                            after_group()
                        proj_fm(sQ, 2 + tgo, ev_q)
                    for tgo in range(2):
                        def ev_g(pi, tgo=tgo):
                            I("act", "activation", out=sg[:, tgo * 512:(tgo + 1) * 512], in_=psA[pi][:], func=AF.Silu,
                              reads=[("psA", pi)], writes=[("sg", tgo)])
                            after_group()
                        proj_fm(sG, 2 + tgo, ev_g)
                    sK = load_w(KA0 + h * 128)
                    sQa = load_w(QA0 + h * 128)
                    sV = load_w(VA0 + h * 128)
                    DMA("sp", MRT[8:10, :], c_arow[h], writes=[("MRT", "a")], key="arow")
                    if h + 1 < nheads:
                        pre = [load_w(IR0 + (h + 1) * 128), load_w(FR0 + (h + 1) * 128)]
                    for tg in range(4):
                        def ev_k(pi, tg=tg):
                            evac_copy(KT[:, tg * 512:(tg + 1) * 512], psA[pi][:], [("psA", pi)], [("KT", tg)])
                            after_group()
                        proj_fm(sK, tg, ev_k)
                    for tgo in range(2):
                        def ev_qa(pi, tgo=tgo):
                            I("act", "mul", out=QT[:, tgo * 512:(tgo + 1) * 512], in_=psA[pi][:], mul=isq,
                              reads=[("psA", pi)], writes=[("QT", tgo)])
                            after_group()
                        proj_fm(sQa, 2 + tgo, ev_qa)
                    for g4 in range(4):
                        pi = next_psA()
                        for k in range(4):
                            i = g4 * 4 + k
                            for c in range(NCH):
                                I("pe", "matmul", psA[pi][:, k * 128:(k + 1) * 128], lhsT=hT[:, c, i * 128:(i + 1) * 128], rhs=Wp[sV][:, c, :],
                                  start=(c == 0), stop=(c == NCH - 1), reads=[("Wp", sV), ("hT", i)], writes=[("psA", pi)])
                        evac_copy(V[:, g4 * 4:(g4 + 1) * 4, :], psA[pi][:].rearrange("p (a b) -> p a b", a=4), [("psA", pi)], [("V", g4)])
                        after_group()
                    while chain:
                        after_group()
                    for tgo in range(2):
                        I("dve", "tensor_tensor", out=qe[:, tgo * 512:(tgo + 1) * 512], in0=sqt[tgo][:], in1=E1[:, tgo * 512:(tgo + 1) * 512],
                          op=ALU.mult, reads=[("sqt", tgo), ("E1",)], writes=[("qe", tgo)])
                    ck(2)
                    for grp in range(4):
                        for k in range(8):
                            c = grp * 8 + k
                            I("pe", "transpose", out=psT[0:64, k, :], in_=KK[:, c * 64:(c + 1) * 64], identity=ident[:],
                              reads=[("KK",), ("ident",)], writes=[("psT", k)])
                        evac_copy(kdT[:, grp * 8:(grp + 1) * 8, :], psT[0:64, :, :], [("psT",)], [("kdT", grp)])

                    def make_u(c):
                        def task():
                            ub, ubn = [(psH, ("psH",)), (psA[0], ("psA", 0)), (psA[1], ("psA", 1))][c % 3]
                            tb = c % 2
                            I("pe", "matmul", ub[:, 0:128], lhsT=kdT[:, c, :], rhs=Vi[:, c, :], start=True, stop=True,
                              reads=[("kdT", c // 8), ("Vi", c // 8)], writes=[ubn])
                            if c == 0:
                                I("dve", "tensor_copy", out=T1[tb][:], in_=ub[:, 0:128], reads=[ubn], writes=[("T1", tb)])
                            else:
                                I("dve", "tensor_tensor", out=T1[tb][:], in0=ub[:, 0:128], in1=Rst[:], op=ALU.add,
                                  reads=[ubn, ("Rst",)], writes=[("T1", tb)])
                            I("dve", "tensor_scalar", out=Rst[:], in0=T1[tb][:], scalar1=gcol[:, c:c + 1], scalar2=None, op0=ALU.mult,
                              reads=[("T1", tb), ("gcol",)], writes=[("Rst",)])
                            if c + 1 >= 16:
                                j = c + 1 - 16
                                I("pool", "tensor_scalar", out=Smb[:, j, :], in0=T1[tb][:], scalar1=gcol[:, c:c + 1], scalar2=None, op0=ALU.mult,
                                  reads=[("T1", tb), ("gcol",)], writes=[("Smb", j)])
                        return task
                    for c in range(31):
                        bg.append(make_u(c))
                    ck(3)
                    ck(4)
                    run_bg(4)
                    ck(5)
                    I("dve", "tensor_reduce", out=km[:], in_=KT[:].rearrange("p (n s) -> p n s", s=256), axis=AX.X, op=ALU.add,
                      reads=[("KT",)], writes=[("km",)])
                    I("dve", "tensor_scalar", out=kmb[:], in0=km[:], scalar1=1.0 / 256, scalar2=None, op0=ALU.mult,
                      reads=[("km",)], writes=[("kmb",)])
                    pg = next_psA()
                    for qt in range(8):
                        I("pe", "matmul", psA[pg][:, qt * 8:(qt + 1) * 8], lhsT=QT[:, qt * 128:(qt + 1) * 128], rhs=kmb[:], start=True, stop=True,
                          reads=[("QT", qt // 4), ("kmb",)], writes=[("psA", pg)])
                    I("dve", "tensor_tensor", out=gm[:], in0=psA[pg][:, 0:64], in1=gmask[:, 2, :], op=ALU.add,
                      reads=[("psA", pg), ("gmask",)], writes=[("gm",)])
                    ck(51)
                    gm3 = gm[:].rearrange("p (q m) -> p q m", m=8)
                    I("dve", "tensor_tensor", out=rinv[:].rearrange("p (q n m) -> p q n m", n=8, m=8),
                      in0=gm3.unsqueeze(2).broadcast_to([128, 8, 8, 8]), in1=gm3.unsqueeze(3).broadcast_to([128, 8, 8, 8]), op=ALU.is_gt,
                      reads=[("gm",)], writes=[("rinv",)])
                    I("dve", "tensor_reduce", out=rank[:], in_=rinv[:].rearrange("p (q m) -> p q m", m=8), axis=AX.X, op=ALU.add,
                      reads=[("rinv",)], writes=[("rank",)])
                    I("dve", "scalar_tensor_tensor", out=rank[:], in0=rank[:], scalar=3.0, in1=gmask[:, 0, :], op0=ALU.is_lt, op1=ALU.mult,
                      reads=[("rank",), ("gmask",)], writes=[("rank",)])
                    I("dve", "tensor_tensor", out=rank[:], in0=rank[:], in1=gmask[:, 1, :], op=ALU.add,
                      reads=[("rank",), ("gmask",)], writes=[("rank",)])
                    I("dve", "tensor_scalar", out=MRq[:], in0=rank[:], scalar1=-1.0, scalar2=-NEG, op0=ALU.add, op1=ALU.mult,
                      reads=[("rank",)], writes=[("MRq",)])
                    ck(52)
                    for qt in range(8):
                        I("pe", "transpose", out=psT[0:8, qt, :], in_=MRq[:, qt * 8:(qt + 1) * 8], identity=ident[:],
                          reads=[("MRq",), ("ident",)], writes=[("psT", qt)])
                    ck(53)
                    evac_copy(MRT[0:8, :].rearrange("p (a b) -> p a b", a=8), psT[0:8, :, :], [("psT",)], [("MRT", "m")])
                    ck(54)
                    run_bg(1)
                    ck(55)
                    run_bg(1)
                    ck(56)
                    run_bg(1)
                    ck(57)
                    run_bg(1)

                    ck(6)
                    if h + 1 < nheads:
                        pre = pre + [load_w(QR0 + (h + 1) * 128), load_w(GR0 + (h + 1) * 128)]
                    steps = [(G, kt) for G in range(2) for kt in range(8 + 4 * G + 4)]

                    def emit_S(si):
                        G, kt = steps[si]
                        sl = si % 2
                        n = kt // 2
                        diag = kt >= 8 + 4 * G
                        I("pe", "matmul", psS[sl][:], lhsT=KT[:, kt * 128:(kt + 1) * 128], rhs=QT[:, G * 512:(G + 1) * 512], start=True, stop=False,
                          reads=[("KT", kt // 4), ("QT", G)], writes=[("psS", sl)])
                        I("pe", "matmul", psS[sl][:], lhsT=En[:, n, :], rhs=MRT[:, G * 512:(G + 1) * 512], start=False, stop=not diag,
                          reads=[("En",), ("MRT",)], writes=[("psS", sl)])
                        if diag:
                            j = kt - 8 - 4 * G
                            I("pe", "matmul", psS[sl][:], lhsT=ident[:], rhs=CB[:, j, :], start=False, stop=True,
                              reads=[("ident",), ("CB",)], writes=[("psS", sl)])
                        pb = si % 3
                        I("act", "activation", out=PT[pb][:], in_=psS[sl][:], func=AF.Exp, bias=bcol[:, h, G, kt:kt + 1],
                          reads=[("psS", sl), ("bcol",)], writes=[("PT", pb)])

                    def emit_PV(si):
                        G, kt = steps[si]
                        pb = si % 3
                        last = kt == 8 + 4 * G + 3
                        I("pe", "matmul", psO[:], lhsT=V[:, kt, :], rhs=PT[pb][:], start=(kt == 0), stop=last,
                          reads=[("V", kt // 4), ("PT", pb)], writes=[("psO",)])
                        I("pe", "matmul", psR[:], lhsT=ones_b[:], rhs=PT[pb][:], start=(kt == 0), stop=last,
                          reads=[("ones_b",), ("PT", pb)], writes=[("psR",)])
                        if last:
                            I("dve", "reciprocal", out=rinv[:], in_=psR[:], reads=[("psR",)], writes=[("rinv",)])
                            I("dve", "tensor_tensor", out=oT[:, h, G * 512:(G + 1) * 512], in0=psO[:], in1=rinv[:], op=ALU.mult,
                              reads=[("psO",), ("rinv",)], writes=[("oT", h, G)])

                    emit_S(0)
                    for si in range(len(steps)):
                        if si + 1 < len(steps):
                            emit_S(si + 1)
                        emit_PV(si)
                        run_bg(1)
                    run_bg(100)

                    ck(7)
                    def emit_AT(j):
                        c = 16 + j
                        sl = j % 8
                        ab = j % 2
                        I("pe", "matmul", psS[ab][0:64, 0:64], lhsT=KK[:, c * 64:(c + 1) * 64], rhs=qe[:, j * 64:(j + 1) * 64],
                          start=True, stop=True, reads=[("KK",), ("qe", j // 8)], writes=[("psS", ab)])
                        I("dve", "tensor_tensor", out=ATs[:, sl, :], in0=psS[ab][0:64, 0:64], in1=m01[:], op=ALU.mult,
                          reads=[("psS", ab), ("m01",)], writes=[("ATs", sl)])

                    def emit_o(j):
                        c = 16 + j
                        sl = j % 8
                        jj = j % 8
                        po = psO if j < 8 else psR
                        pon = "psO" if j < 8 else "psR"
                        I("pe", "matmul", po[:, jj * 64:(jj + 1) * 64], lhsT=Smb[:, j, :], rhs=qe[:, j * 64:(j + 1) * 64], start=True, stop=False,
                          reads=[("Smb", j), ("qe", j // 8)], writes=[(pon, jj)])
                        I("pe", "matmul", po[:, jj * 64:(jj + 1) * 64], lhsT=Vi[:, c, :], rhs=ATs[:, sl, :], start=False, stop=True,
                          reads=[("Vi", c // 8), ("ATs", sl)], writes=[(pon, jj)])
                        if jj == 7:
                            hh = j // 8
                            evac_copy(OT[:, hh * 512:(hh + 1) * 512], po[:], [(pon,)], [("OT", hh)])

                    emit_AT(0)
                    emit_AT(1)
                    for j in range(16):
                        if j + 2 < 16:
                            emit_AT(j + 2)
                        emit_o(j)
                    ck(8)
                    I("act", "activation", out=sqo[:], in_=OT[:], func=AF.Square, reads=[("OT",)], writes=[("E1",)])
                    for hh in range(2):
                        pi = next_psA()
                        I("pe", "matmul", psA[pi][:], lhsT=ones_b[:], rhs=sqo[:, hh * 512:(hh + 1) * 512], start=True, stop=True,
                          reads=[("ones_b",), ("E1",)], writes=[("psA", pi)])
                        I("act", "activation", out=rsn[:], in_=psA[pi][:], func=AF.Sqrt, bias=epsc[:], scale=1.0 / 128,
                          reads=[("psA", pi), ("epsc",)], writes=[("rsn",)])
                        I("dve", "reciprocal", out=rsn[:], in_=rsn[:], reads=[("rsn",)], writes=[("rsn",)])
                        I("dve", "scalar_tensor_tensor", out=OT[:, hh * 512:(hh + 1) * 512], in0=OT[:, hh * 512:(hh + 1) * 512],
                          scalar=ong[:, 0:1], in1=rsn[:], op0=ALU.mult, op1=ALU.mult,
                          reads=[("OT", hh), ("ong",), ("rsn",)], writes=[("OT", hh)])
                        I("dve", "tensor_tensor", out=oT[:, 8 + h, hh * 512:(hh + 1) * 512], in0=OT[:, hh * 512:(hh + 1) * 512],
                          in1=sg[:, hh * 512:(hh + 1) * 512], op=ALU.mult,
                          reads=[("OT", hh), ("sg", hh)], writes=[("oT", 8 + h, hh)])
                    P.maybe_barrier(int(_os.environ.get("KBAR", "900")))

                if debug:
                    DMA("sp", dbg["hT"], hT[:].rearrange("p a b -> p (a b)"), reads=[("hT",)], key="dbg0")
                    DMA("sp", dbg["oT"], oT[:].rearrange("p a b -> p (a b)"), reads=[("oT",)], key="dbg1")
                if upto == 2:
                    P.emit()
                    raise _Stop(nc)
                P.barrier()

        x1 = T_(nc.sbuf_tensor("x1", [128, 8, D], F32))
        h2T = T_(nc.sbuf_tensor("h2T", [128, NCH, TO], BF16))
        comb = T_(nc.sbuf_tensor("comb", [128, 8, 16], F32))
        gB = T_(nc.sbuf_tensor("gB3", [128, D], F32))
        with contextlib.ExitStack() as s3:
            S_ = s3.enter_context
            Wo = [S_(nc.sbuf_tensor("Wo%d" % i, [128, NCH, 512], BF16)) for i in range(2)]
            xr = [S_(nc.sbuf_tensor("xr%d" % i, [128, 512], F32)) for i in range(3)]
            hn2 = [S_(nc.sbuf_tensor("hn2_%d" % i, [128, D], BF16)) for i in range(2)]
            wrb = S_(nc.sbuf_tensor("wrb", [128, NCH, 20], BF16))
            brB = S_(nc.sbuf_tensor("brB", [128, 20], F32))
            lg = S_(nc.sbuf_tensor("lg", [128, 8, 20], F32))
            rt = {}
            for nm, shp in [("gmax", [128, 8]), ("gsh", [128, 8, 4]), ("gsum", [128, 8]), ("gone", [128, 8, 4]),
                            ("t16", [128, 8, 16]), ("els", [128, 8, 4]), ("els2", [128, 8, 4]), ("e1", [128, 8]),
                            ("e2", [128, 8]), ("m1", [128, 8, 4]), ("m2", [128, 8, 4]), ("r", [128, 8]),
                            ("w1", [128, 8]), ("w2", [128, 8]), ("ew", [128, 8, 4])]:
                rt[nm] = S_(nc.sbuf_tensor("r_" + nm, shp, F32))

            DMA("sp", gB[:], g_ffn.broadcast_to([128, D]), writes=[("gB",)], key="gB")
            DMA("pool", wrb[:], wr.rearrange("(c p) n -> p c n", p=128), writes=[("wrb",)], key="wrb")
            DMA("sp", brB[:], br.broadcast_to([128, 20]), writes=[("brB",)], key="brB")
            xrc = 0
            for db in range(4):
                ws = db % 2
                DMA("pool", Wo[ws][:], w_out[:, db * 512:(db + 1) * 512].rearrange("(c p) n -> p c n", p=128), writes=[("Wo", ws)], key=("Wo", ws))
                for i in range(8):
                    xs = xrc % 3
                    xrc += 1
                    DMA("sp", xr[xs][:], xc[TO + i * 128:TO + (i + 1) * 128, db * 512:(db + 1) * 512], writes=[("xr", xs)], key=("xr", xs))
                    pi = next_psA()
                    for c in range(NCH):
                        I("pe", "matmul", psA[pi][:], lhsT=oT[:, c, i * 128:(i + 1) * 128], rhs=Wo[ws][:, c, :], start=(c == 0), stop=(c == NCH - 1),
                          reads=[("oT",), ("Wo", ws)], writes=[("psA", pi)])
                    I("dve", "tensor_tensor", out=x1[:, i, db * 512:(db + 1) * 512], in0=psA[pi][:], in1=xr[xs][:], op=ALU.add,
                      reads=[("psA", pi), ("xr", xs)], writes=[("x1", i, db)])
            for i in range(8):
                b_ = i % 2
                rms_tile(gB, x1[:, i, :], [("x1", i)], 16 + i, hn2[b_][:], ("hn2", b_), hn2[b_][:], ("hn2", b_))
                for half in range(2):
                    for c in range(8):
                        cc = half * 8 + c
                        I("pe", "transpose", out=psT[:, c, :], in_=hn2[b_][:, cc * 128:(cc + 1) * 128], identity=ident[:],
                          reads=[("hn2", b_), ("ident",)], writes=[("psT", c)])
                    evac_copy(h2T[:, half * 8:(half + 1) * 8, i * 128:(i + 1) * 128], psT[:], [("psT",)], [("h2T", i)])
                pi = next_psA()
                for c in range(NCH):
                    I("pe", "matmul", psA[pi][:, 0:20], lhsT=h2T[:, c, i * 128:(i + 1) * 128], rhs=wrb[:, c, :], start=(c == 0), stop=(c == NCH - 1),
                      reads=[("h2T", i), ("wrb",)], writes=[("psA", pi)])
                I("dve", "tensor_tensor", out=lg[:, i, :], in0=psA[pi][:, 0:20], in1=brB[:], op=ALU.add,
                  reads=[("psA", pi), ("brB",)], writes=[("lg", i)])
            gl = lg[:, :, 0:4]
            el = lg[:, :, 4:20].rearrange("p q (g e) -> p q g e", g=4)

            def RT(*names):
                return [(n,) for n in names]
            b84 = lambda ap: ap.unsqueeze(2).broadcast_to([128, 8, 4])
            I("dve", "tensor_reduce", out=rt["gmax"][:], in_=gl, axis=AX.X, op=ALU.max, reads=RT("lg"), writes=RT("gmax"))
            I("dve", "tensor_tensor", out=rt["gsh"][:], in0=gl, in1=b84(rt["gmax"][:]), op=ALU.subtract, reads=RT("lg", "gmax"), writes=RT("gsh"))
            I("dve", "tensor_scalar", out=rt["gone"][:], in0=rt["gsh"][:], scalar1=0.0, scalar2=None, op0=ALU.is_ge, reads=RT("gsh"), writes=RT("gone"))
            I("act", "activation", out=rt["gsh"][:], in_=rt["gsh"][:], func=AF.Exp, reads=RT("gsh"), writes=RT("gsh"))
            I("dve", "tensor_reduce", out=rt["gsum"][:], in_=rt["gsh"][:], axis=AX.X, op=ALU.add, reads=RT("gsh"), writes=RT("gsum"))
            I("dve", "reciprocal", out=rt["gsum"][:], in_=rt["gsum"][:], reads=RT("gsum"), writes=RT("gsum"))
            I("dve", "tensor_tensor", out=rt["t16"][:].rearrange("p q (g e) -> p q g e", g=4), in0=el,
              in1=rt["gone"][:].unsqueeze(3).broadcast_to([128, 8, 4, 4]), op=ALU.mult, reads=RT("lg", "gone"), writes=RT("t16"))
            I("dve", "tensor_reduce", out=rt["els"][:], in_=rt["t16"][:].rearrange("p q (g e) -> p q e g", g=4), axis=AX.X, op=ALU.add,
              reads=RT("t16"), writes=RT("els"))
            I("dve", "tensor_reduce", out=rt["e1"][:], in_=rt["els"][:], axis=AX.X, op=ALU.max, reads=RT("els"), writes=RT("e1"))
            I("dve", "tensor_tensor", out=rt["m1"][:], in0=rt["els"][:], in1=b84(rt["e1"][:]), op=ALU.is_ge, reads=RT("els", "e1"), writes=RT("m1"))
            I("dve", "scalar_tensor_tensor", out=rt["els2"][:], in0=rt["m1"][:], scalar=-1e30, in1=rt["els"][:], op0=ALU.mult, op1=ALU.add,
              reads=RT("m1", "els"), writes=RT("els2"))
            I("dve", "tensor_reduce", out=rt["e2"][:], in_=rt["els2"][:], axis=AX.X, op=ALU.max, reads=RT("els2"), writes=RT("e2"))
            I("dve", "tensor_tensor", out=rt["m2"][:], in0=rt["els2"][:], in1=b84(rt["e2"][:]), op=ALU.is_ge, reads=RT("els2", "e2"), writes=RT("m2"))
            I("dve", "tensor_tensor", out=rt["r"][:], in0=rt["e2"][:], in1=rt["e1"][:], op=ALU.subtract, reads=RT("e1", "e2"), writes=RT("r"))
            I("act", "activation", out=rt["r"][:], in_=rt["r"][:], func=AF.Exp, reads=RT("r"), writes=RT("r"))
            I("dve", "tensor_scalar", out=rt["w1"][:], in0=rt["r"][:], scalar1=1.0, scalar2=None, op0=ALU.add, reads=RT("r"), writes=RT("w1"))
            I("dve", "reciprocal", out=rt["w1"][:], in_=rt["w1"][:], reads=RT("w1"), writes=RT("w1"))
            I("dve", "tensor_tensor", out=rt["w1"][:], in0=rt["w1"][:], in1=rt["gsum"][:], op=ALU.mult, reads=RT("w1", "gsum"), writes=RT("w1"))
            I("dve", "tensor_tensor", out=rt["w2"][:], in0=rt["w1"][:], in1=rt["r"][:], op=ALU.mult, reads=RT("w1", "r"), writes=RT("w2"))
            I("dve", "tensor_tensor", out=rt["m1"][:], in0=rt["m1"][:], in1=b84(rt["w1"][:]), op=ALU.mult, reads=RT("m1", "w1"), writes=RT("m1"))
            I("dve", "tensor_tensor", out=rt["m2"][:], in0=rt["m2"][:], in1=b84(rt["w2"][:]), op=ALU.mult, reads=RT("m2", "w2"), writes=RT("m2"))
            I("dve", "tensor_tensor", out=rt["ew"][:], in0=rt["m1"][:], in1=rt["m2"][:], op=ALU.add, reads=RT("m1", "m2"), writes=RT("ew"))
            I("dve", "tensor_tensor", out=comb[:].rearrange("p q (g e) -> p q g e", g=4),
              in0=rt["gone"][:].unsqueeze(3).broadcast_to([128, 8, 4, 4]), in1=rt["ew"][:].unsqueeze(2).broadcast_to([128, 8, 4, 4]),
              op=ALU.mult, reads=RT("gone", "ew"), writes=RT("comb"))
            if debug:
                DMA("sp", dbg["x1"], x1[:].rearrange("p a b -> p (a b)"), reads=[("x1",)], key="dbg2")
                DMA("sp", dbg["comb"], comb[:].rearrange("p a b -> p (a b)"), reads=[("comb",)], key="dbg3")
            if upto == 3:
                P.emit()
                raise _Stop(nc)
            P.barrier()

        with contextlib.ExitStack() as s4:
            S_ = s4.enter_context
            Wgu = [S_(nc.sbuf_tensor("Wgu%d" % i, [128, NCH, 256], BF16)) for i in range(4)]
            Wd = [S_(nc.sbuf_tensor("Wd%d" % i, [128, 8, 512], BF16)) for i in range(2)]
            hidT = oT[:, 0:8, :]
            sgt = [oT[:, 8, 0:512], oT[:, 8, 512:1024]]
            junk = oT[:, 10:12, :].rearrange("p a b -> p (a b)")
            DMA("sp", gB[:], g_fin.broadcast_to([128, D]), writes=[("gB",)], key="gB")
            gus = 0
            dsl = 0
            psGU = [psA[0], psA[1], psS[0], psS[1]]
            psGUn = [("psA", 0), ("psA", 1), ("psS", 0), ("psS", 1)]
            psY = [psO, psR]
            psYn = [("psO",), ("psR",)]
            guc = 0
            yc = 0
            sgc = 0
            for ex in range(NE):
                for fq in range(4):
                    sg_ = gus % 4
                    su_ = (gus + 1) % 4
                    gus += 2
                    DMA("pool", Wgu[sg_][:], w_gate[ex, :, fq * 256:(fq + 1) * 256].rearrange("(c p) n -> p c n", p=128),
                        writes=[("Wgu", sg_)], key=("Wgu", sg_))
                    DMA("pool", Wgu[su_][:], w_up[ex, :, fq * 256:(fq + 1) * 256].rearrange("(c p) n -> p c n", p=128),
                        writes=[("Wgu", su_)], key=("Wgu", su_))
                    for fs in range(2):
                        fb = fq * 2 + fs
                        for tg in range(2):
                            ig = guc % 4
                            iu = (guc + 1) % 4
                            guc += 2
                            for c in range(NCH):
                                I("pe", "matmul", psGU[ig][:], lhsT=Wgu[sg_][:, c, fs * 128:(fs + 1) * 128], rhs=h2T[:, c, tg * 512:(tg + 1) * 512],
                                  start=(c == 0), stop=(c == NCH - 1), reads=[("Wgu", sg_), ("h2T",)], writes=[psGUn[ig]])
                            for c in range(NCH):
                                I("pe", "matmul", psGU[iu][:], lhsT=Wgu[su_][:, c, fs * 128:(fs + 1) * 128], rhs=h2T[:, c, tg * 512:(tg + 1) * 512],
                                  start=(c == 0), stop=(c == NCH - 1), reads=[("Wgu", su_), ("h2T",)], writes=[psGUn[iu]])
                            sb = sgc % 2
                            sgc += 1
                            I("act", "activation", out=sgt[sb], in_=psGU[ig][:], func=AF.Silu, reads=[psGUn[ig]], writes=[("sgt", sb)])
                            I("dve", "tensor_tensor", out=hidT[:, fb, tg * 512:(tg + 1) * 512], in0=psGU[iu][:], in1=sgt[sb], op=ALU.mult,
                              reads=[psGUn[iu], ("sgt", sb)], writes=[("hidT", fb, tg)])
                for db in range(4):
                    ds_ = dsl % 2
                    dsl += 1
                    DMA("pool", Wd[ds_][:], w_down[ex, :, db * 512:(db + 1) * 512].rearrange("(c p) n -> p c n", p=128),
                        writes=[("Wd", ds_)], key=("Wd", ds_))
                    for i in range(8):
                        iy = yc % 2
                        yc += 1
                        for fb in range(8):
                            I("pe", "matmul", psY[iy][:], lhsT=hidT[:, fb, i * 128:(i + 1) * 128], rhs=Wd[ds_][:, fb, :], start=(fb == 0), stop=(fb == 7),
                              reads=[("hidT", fb, i // 4), ("Wd", ds_)], writes=[psYn[iy]])
                        I("dve", "scalar_tensor_tensor", out=x1[:, i, db * 512:(db + 1) * 512], in0=psY[iy][:], scalar=comb[:, i, ex:ex + 1],
                          in1=x1[:, i, db * 512:(db + 1) * 512], op0=ALU.mult, op1=ALU.add,
                          reads=[psYn[iy], ("comb",), ("x1", i, db)], writes=[("x1", i, db)])
                P.maybe_barrier(900)
            for i in range(8):
                I("act", "activation", out=junk, in_=x1[:, i, :], func=AF.Square, accum_out=ss[:, 24 + i:25 + i],
                  reads=[("x1", i)], writes=[("junk",), ("ss", 24 + i)])
                I("act", "activation", out=rstd[:, 24 + i:25 + i], in_=ss[:, 24 + i:25 + i], func=AF.Sqrt, bias=epsc[:], scale=1.0 / D,
                  reads=[("ss", 24 + i), ("epsc",)], writes=[("rstd", 24 + i)])
                I("dve", "reciprocal", out=rstd[:, 24 + i:25 + i], in_=rstd[:, 24 + i:25 + i], reads=[("rstd", 24 + i)], writes=[("rstd", 24 + i)])
                I("dve", "scalar_tensor_tensor", out=x1[:, i, :], in0=x1[:, i, :], scalar=rstd[:, 24 + i:25 + i], in1=gB[:],
                  op0=ALU.mult, op1=ALU.mult, reads=[("x1", i), ("rstd", 24 + i), ("gB",)], writes=[("x1", i)])
                DMA("sp", y_out[i * 128:(i + 1) * 128, :], x1[:, i, :], reads=[("x1", i)], key=("yo", i % 2))
            P.emit()
    return nc


def _consts(half):
    bf = ml_dtypes.bfloat16
    slopes = 2.0 ** (-8.0 * np.arange(1, NH + 1) / NH)
    c = {}
    c["c_ident"] = np.eye(128, dtype=np.float32).astype(bf)
    gmk = np.zeros((3, 8, 8), np.float32)
    nmin = 0 if half == 1 else 4
    for qt in range(8):
        qb = 4 + qt // 2
        for n in range(8):
            past = (n < qb) and (n >= nmin)
            gmk[0, qt, n] = 1.0 if past else 0.0
            gmk[1, qt, n] = 1.0 if n == qb else 0.0
            gmk[2, qt, n] = 0.0 if past else -1e30
    c["c_gmask"] = np.ascontiguousarray(np.broadcast_to(gmk.reshape(1, 192), (128, 192))).astype(np.float32)
    en = np.zeros((128, 8, 128), np.float32)
    for n in range(8):
        en[n, n, :] = 1.0
        en[8, n, :] = 1.0
        en[9, n, :] = 1.0
    c["c_en"] = en.reshape(128, 1024).astype(bf)
    cb = np.zeros((128, 4, 512), np.float32)
    cc = np.arange(128)[:, None]
    tt = np.arange(512)[None, :]
    for j in range(4):
        cb[:, j, :] = np.where(tt >= 128 * j + cc, 0.0, NEG)
    c["c_cb"] = cb.reshape(128, 2048).astype(bf)
    bc = np.zeros((128, 8, 2, 16), np.float32)
    p = np.arange(128)
    for h in range(8):
        for G in range(2):
            for kt in range(16):
                bc[:, h, G, kt] = -slopes[h] * (1024 + 512 * G - (128 * kt + p))
    c["c_bcol"] = bc.reshape(128, 256).astype(np.float32)
    ar = np.zeros((8, 2, 1024), np.float32)
    trel = np.arange(1024) % 512
    for h in range(8):
        ar[h, 0] = -slopes[h] * (128 * (trel // 128))
        ar[h, 1] = -slopes[h] * (trel % 128)
    c["c_arow"] = ar.astype(bf)
    m01 = (np.arange(64)[:, None] <= np.arange(64)[None, :]).astype(np.float32)
    c["c_m01"] = m01
    return c


_NC_CACHE = {}


def kernel(x, norm_mix_g, w_in, hgrn_lb_logits, hgrn_out_norm_g, w_out, norm_ffn_g,
           w_group_router, b_group_router, w_expert_router, b_expert_router,
           w_gate, w_up, w_down, final_norm_g, _debug=False, _upto=9, _nheads=NH, _ncores=8):
    f32 = np.float32
    x = np.asarray(x, f32)
    B = x.shape[0]
    w_in0 = np.ascontiguousarray(np.asarray(w_in, f32)[0])
    w_out0 = np.ascontiguousarray(np.asarray(w_out, f32)[0])
    wg = np.ascontiguousarray(np.asarray(w_gate, f32)[0])
    wu = np.ascontiguousarray(np.asarray(w_up, f32)[0])
    wd = np.ascontiguousarray(np.asarray(w_down, f32)[0])
    wgr = np.asarray(w_group_router, f32)[0]
    wer = np.asarray(w_expert_router, f32)[0]
    wr = np.ascontiguousarray(np.concatenate([wgr] + [wer[g] for g in range(4)], axis=1))
    br = np.ascontiguousarray(np.concatenate([np.asarray(b_group_router, f32)[0].reshape(-1),
                                              np.asarray(b_expert_router, f32)[0].reshape(-1)]).reshape(1, 20))
    lbl = np.asarray(hgrn_lb_logits, f32)
    lbl_t = np.ascontiguousarray(lbl.reshape(2, 8, 128).transpose(2, 0, 1).reshape(128, 16))
    ong = np.ascontiguousarray(np.asarray(hgrn_out_norm_g, f32)[0].reshape(128, 1))
    g_mix = np.ascontiguousarray(np.asarray(norm_mix_g, f32)[0].reshape(1, D))
    g_ffn = np.ascontiguousarray(np.asarray(norm_ffn_g, f32)[0].reshape(1, D))
    g_fin = np.ascontiguousarray(np.asarray(final_norm_g, f32).reshape(1, D))

    key = (bool(_debug), _upto, _nheads)
    if key not in _NC_CACHE:
        _NC_CACHE[key] = build_nc(debug=_debug, upto=_upto, nheads=_nheads)
    nc = _NC_CACHE[key]
    consts = [_consts(0), _consts(1)]
    in_maps = []
    for core in range(_ncores):
        b, half = core // 2, core % 2
        xcore = np.zeros((TT, D), f32)
        if half == 1:
            xcore[:TO] = x[b, :TO]
            xcore[TO:] = x[b, TO:]
        else:
            xcore[TO:] = x[b, :TO]
        m = dict(xc=xcore, w_in=w_in0, w_out=w_out0, w_gate=wg, w_up=wu, w_down=wd, wr=wr, br=br,
                 g_mix=g_mix, g_ffn=g_ffn, g_fin=g_fin, lbl=lbl_t, ong=ong)
        m.update(consts[half])
        in_maps.append(m)
    res = run_bass_kernel_spmd(nc, in_maps, core_ids=list(range(_ncores)))
    out = np.zeros((B, 2048, D), f32)
    for core in range(_ncores):
        b, half = core // 2, core % 2
        out[b, half * TO:(half + 1) * TO] = res.results[core]["y"]
    if _debug:
        return out, res.results
    return out
```

```python
import contextlib
import numpy as np
import ml_dtypes
import concourse.bass as bass
import concourse.mybir as mybir
from concourse.bass_utils import run_bass_kernel_spmd

F32 = mybir.dt.float32
BF16 = mybir.dt.bfloat16
AF = mybir.ActivationFunctionType
ALU = mybir.AluOpType
AX = mybir.AxisListType

ENGS = ("pe", "act", "dve", "pool", "sp")


class Op:
    __slots__ = ("eng", "fn", "reads", "writes", "dma", "key", "sig", "sigval",
                 "idx", "dmaval", "waits", "epoch")

    def __init__(self, eng, fn, reads, writes, dma, key, epoch):
        self.eng = eng
        self.fn = fn
        self.reads = reads
        self.writes = writes
        self.dma = dma
        self.key = key
        self.sig = False
        self.sigval = 0
        self.dmaval = 0
        self.waits = []
        self.epoch = epoch


class Prog:
    def __init__(self, nc, same_engine_sync=("act", "dve", "pool")):
        self.nc = nc
        self.ops = []
        self.same = set(same_engine_sync)
        self.state = {}
        self.children = {}
        self.dma_count = {}
        self.epoch = 0
        self.nsig = {e: 0 for e in ENGS}

    def _related(self, tok):
        out = []
        for i in range(1, len(tok) + 1):
            p = tok[:i]
            if p in self.state:
                out.append(p)
        for t in self.children.get(tok, ()):
            out.append(t)
        return out

    def _touch(self, tok):
        if tok not in self.state:
            self.state[tok] = [None, []]
            for i in range(1, len(tok)):
                self.children.setdefault(tok[:i], set()).add(tok)
        return self.state[tok]

    def add(self, eng, fn, reads=(), writes=(), dma=False, key=None):
        op = Op(eng, fn, [tuple(r) for r in reads], [tuple(w) for w in writes], dma, key, self.epoch)
        op.idx = len(self.ops)
        deps = {}
        for r in op.reads:
            for t in self._related(r):
                w = self.state[t][0]
                if w is not None:
                    deps[id(w)] = w
        for wtok in op.writes:
            for t in self._related(wtok):
                st = self.state[t]
                if st[0] is not None:
                    deps[id(st[0])] = st[0]
                for rd in st[1]:
                    deps[id(rd)] = rd
        for d in deps.values():
            self._dep(op, d)
        for r in op.reads:
            rl = self._touch(r)[1]
            if not dma:
                rl[:] = [o for o in rl if o.dma or o.eng != eng]
            rl.append(op)
        for wtok in op.writes:
            st = self._touch(wtok)
            st[0] = op
            st[1] = []
            for t in list(self.children.get(wtok, ())):
                self.state[t] = [op, []]
        if dma:
            self.dma_count[key] = self.dma_count.get(key, 0) + 16
            op.dmaval = self.dma_count[key]
        self.ops.append(op)
        return op

    def _dep(self, op, d):
        if d.dma:
            op.waits.append(("dma", d.key, self.dma_count[d.key]))
        else:
            if d.eng == op.eng and not op.dma and d.eng not in self.same:
                return
            if not d.sig:
                d.sig = True
                self.nsig[d.eng] += 1
            op.waits.append(("eng", d))

    def barrier(self):
        last_nd = {}
        for o in reversed(self.ops):
            if o.epoch != self.epoch:
                break
            if not o.dma and o.eng not in last_nd:
                last_nd[o.eng] = o
            if len(last_nd) == len(ENGS):
                break
        dkeys = dict(self.dma_count)
        for eng in ENGS:
            def nopfn(e):
                return e.nop()
            op = Op(eng, nopfn, [], [], False, None, self.epoch)
            op.idx = len(self.ops)
            for o in last_nd.values():
                if o.eng == eng and eng not in self.same:
                    continue
                if not o.sig:
                    o.sig = True
                op.waits.append(("eng", o))
            for k, v in dkeys.items():
                op.waits.append(("dma", k, v))
            self.ops.append(op)
        self.state = {}
        self.children = {}
        self.epoch += 1
        self.nsig = {e: 0 for e in ENGS}

    def maybe_barrier(self, limit):
        if max(self.nsig.values()) > limit:
            self.barrier()

    def emit(self):
        nc = self.nc
        cnt = {}
        for op in self.ops:
            if op.sig and not op.dma:
                k = (op.eng, op.epoch)
                cnt[k] = cnt.get(k, 0) + 1
                op.sigval = cnt[k]
        self.sigcounts = cnt
        keys = sorted(self.dma_count.keys(), key=str)
        with contextlib.ExitStack() as st:
            esem = {}
            for ep in range(self.epoch + 1):
                for e in ENGS:
                    if (e, ep) in cnt:
                        esem[(e, ep)] = st.enter_context(nc.semaphore("s_%s_%d" % (e, ep)))
            dsem = {k: st.enter_context(nc.semaphore("d_%d" % i)) for i, k in enumerate(keys)}
            self.nsems = len(esem) + len(dsem)
            block = st.enter_context(nc.Block())

            def make(engname):
                def body(e):
                    waited = {}
                    for op in self.ops:
                        if op.eng != engname:
                            continue
                        for w in op.waits:
                            if w[0] == "dma":
                                sem, val, kk = dsem[w[1]], w[2], ("d", w[1])
                            else:
                                kk = (w[1].eng, w[1].epoch)
                                sem, val = esem[kk], w[1].sigval
                            if waited.get(kk, 0) >= val:
                                continue
                            waited[kk] = val
                            e.wait_ge(sem, val)
                        ins = op.fn(e)
                        if op.dma:
                            ins.then_inc(dsem[op.key], 16)
                        elif op.sig:
                            ins.then_inc(esem[(op.eng, op.epoch)], 1)
                    if engname == "sp":
                        for k in keys:
                            e.wait_ge(dsem[k], self.dma_count[k])
                return body

            block.tensor(make("pe"))
            block.scalar(make("act"))
            block.vector(make("dve"))
            block.gpsimd(make("pool"))
            block.sync(make("sp"))


D = 2048
TT = 2048
TO = 1024
NCH = 16
NH = 8
DH = 128
INC = 7168
NE = 16
DE = 1024
EPS = 1e-6
NEG = -30000.0
QA0, KA0, VA0, QR0, FR0, IR0, GR0 = 0, 1024, 2048, 3072, 4096, 5120, 6144


class _Stop(Exception):
    pass


def build_nc(debug=False, upto=9, nheads=NH):
    try:
        return _build_nc(debug, upto, nheads)
    except _Stop as s:
        return s.args[0]


def _build_nc(debug, upto, nheads):
    nc = bass.Bass("TRN2", target_bir_lowering=False)

    def din(name, shape, dt=F32):
        return nc.dram_tensor(name, list(shape), dt, kind="ExternalInput").ap()

    xc = din("xc", [TT, D])
    w_in = din("w_in", [D, INC])
    w_out = din("w_out", [D, D])
    w_gate = din("w_gate", [NE, D, DE])
    w_up = din("w_up", [NE, D, DE])
    w_down = din("w_down", [NE, DE, D])
    wr = din("wr", [D, 20])
    br = din("br", [1, 20])
    g_mix = din("g_mix", [1, D])
    g_ffn = din("g_ffn", [1, D])
    g_fin = din("g_fin", [1, D])
    lbl = din("lbl", [128, 16])
    ong_d = din("ong", [128, 1])
    c_ident = din("c_ident", [128, 128], BF16)
    c_gmask = din("c_gmask", [128, 3 * 64])
    c_en = din("c_en", [128, 8 * 128], BF16)
    c_cb = din("c_cb", [128, 4 * 512], BF16)
    c_bcol = din("c_bcol", [128, 8 * 2 * 16])
    c_arow = din("c_arow", [8, 2, 1024], BF16)
    c_m01 = din("c_m01", [64, 64])
    y_out = nc.dram_tensor("y", [TO, D], F32, kind="ExternalOutput").ap()
    dbg = {}
    if debug:
        dbg["oT"] = nc.dram_tensor("dbg_oT", [128, 16 * TO], BF16, kind="ExternalOutput").ap()
        dbg["x1"] = nc.dram_tensor("dbg_x1", [128, 8 * D], F32, kind="ExternalOutput").ap()
        dbg["comb"] = nc.dram_tensor("dbg_comb", [128, 8 * 16], F32, kind="ExternalOutput").ap()
        dbg["hT"] = nc.dram_tensor("dbg_hT", [128, 16 * TT], BF16, kind="ExternalOutput").ap()

    P = Prog(nc)

    def I(eng, name, *args, reads=(), writes=(), dma=False, key=None, **kw):
        def fn(e, name=name, args=args, kw=kw):
            return getattr(e, name)(*args, **kw)
        return P.add(eng, fn, reads, writes, dma, key)

    def DMA(eng, out, in_, reads=(), writes=(), key=None):
        return I(eng, "dma_start", out=out, in_=in_, reads=reads, writes=writes, dma=True, key=key)

    isq = float(DH) ** -0.5

    with contextlib.ExitStack() as top:
        T_ = top.enter_context
        psA = [T_(nc.psum_tensor("psA%d" % i, [128, 512], F32)) for i in range(2)]
        psS = [T_(nc.psum_tensor("psS%d" % i, [128, 512], F32)) for i in range(2)]
        psO = T_(nc.psum_tensor("psO", [128, 512], F32))
        psR = T_(nc.psum_tensor("psR", [128, 512], F32))
        psT = T_(nc.psum_tensor("psT", [128, 8, 128], BF16))
        psH = T_(nc.psum_tensor("psH", [128, 512], F32))
        oT = T_(nc.sbuf_tensor("oT", [128, 16, TO], BF16))
        ident = T_(nc.sbuf_tensor("ident", [128, 128], BF16))
        ones_b = T_(nc.sbuf_tensor("ones_b", [128, 128], BF16))
        epsc = T_(nc.sbuf_tensor("epsc", [128, 1], F32))
        ss = T_(nc.sbuf_tensor("ss", [128, 32], F32))
        rstd = T_(nc.sbuf_tensor("rstd", [128, 32], F32))

        DMA("sp", ident[:], c_ident, writes=[("ident",)], key="c0")
        I("dve", "memset", ones_b[:], 1.0, writes=[("ones_b",)])
        I("dve", "memset", epsc[:], EPS, writes=[("epsc",)])

        acc_ctr = [0]

        def next_psA():
            i = acc_ctr[0] % 2
            acc_ctr[0] += 1
            return i

        cp_ctr = [0]

        def evac_copy(out, in_, reads, writes):
            i = cp_ctr[0] % 2
            cp_ctr[0] += 1
            if i == 0:
                I("act", "copy", out=out, in_=in_, reads=reads, writes=writes)
            else:
                I("dve", "tensor_copy", out=out, in_=in_, reads=reads, writes=writes)

        def rms_tile(gB, src_ap, srctoks, idx, hn_ap, hn_tok, junk_ap, junk_tok):
            I("act", "activation", out=junk_ap, in_=src_ap, func=AF.Square, accum_out=ss[:, idx:idx + 1],
              reads=srctoks, writes=[junk_tok, ("ss", idx)])
            I("act", "activation", out=rstd[:, idx:idx + 1], in_=ss[:, idx:idx + 1], func=AF.Sqrt, bias=epsc[:], scale=1.0 / D,
              reads=[("ss", idx), ("epsc",)], writes=[("rstd", idx)])
            I("dve", "reciprocal", out=rstd[:, idx:idx + 1], in_=rstd[:, idx:idx + 1], reads=[("rstd", idx)], writes=[("rstd", idx)])
            I("dve", "scalar_tensor_tensor", out=hn_ap, in0=src_ap, scalar=rstd[:, idx:idx + 1], in1=gB[:], op0=ALU.mult, op1=ALU.mult,
              reads=list(srctoks) + [("rstd", idx), ("gB",)], writes=[hn_tok])

        with contextlib.ExitStack() as sHT:
            hT = sHT.enter_context(nc.sbuf_tensor("hT", [128, NCH, TT], BF16))
            with contextlib.ExitStack() as s1:
                S_ = s1.enter_context
                gB = S_(nc.sbuf_tensor("gB1", [128, D], F32))
                xt = [S_(nc.sbuf_tensor("xt%d" % i, [128, D], F32)) for i in range(4)]
                hn = [S_(nc.sbuf_tensor("hn%d" % i, [128, D], BF16)) for i in range(4)]
                DMA("sp", gB[:], g_mix.broadcast_to([128, D]), writes=[("gB",)], key="gB")
                def p1_stage1(i):
                    b_ = i % 4
                    DMA("sp", xt[b_][:], xc[i * 128:(i + 1) * 128, :], writes=[("xt", b_)], key=("xt", b_))
                    rms_tile(gB, xt[b_][:], [("xt", b_)], i, hn[b_][:], ("hn", b_), hn[b_][:], ("hn", b_))

                def p1_stage2(i):
                    b_ = i % 4
                    for half in range(2):
                        for c in range(8):
                            cc = half * 8 + c
                            I("pe", "transpose", out=psT[:, c, :], in_=hn[b_][:, cc * 128:(cc + 1) * 128], identity=ident[:],
                              reads=[("hn", b_), ("ident",)], writes=[("psT", c)])
                        evac_copy(hT[:, half * 8:(half + 1) * 8, i * 128:(i + 1) * 128], psT[:], [("psT",)], [("hT", i)])

                p1_stage1(0)
                p1_stage1(1)
                for i in range(16):
                    if i + 2 < 16:
                        p1_stage1(i + 2)
                    p1_stage2(i)
                if upto == 1:
                    DMA("sp", dbg["hT"], hT[:].rearrange("p a b -> p (a b)"), reads=[("hT",)], key="dbg0")
                    P.emit()
                    raise _Stop(nc)
                P.barrier()

            with contextlib.ExitStack() as s2:
                S_ = s2.enter_context
                NWP = 6
                Wp = [S_(nc.sbuf_tensor("Wp%d" % i, [128, NCH, 128], BF16)) for i in range(NWP)]
                QT = S_(nc.sbuf_tensor("QT", [128, TO], BF16))
                KT = S_(nc.sbuf_tensor("KT", [128, TT], BF16))
                V = S_(nc.sbuf_tensor("V", [128, 16, 128], BF16))
                MRT = S_(nc.sbuf_tensor("MRT", [128, TO], BF16))
                PT = [S_(nc.sbuf_tensor("PT%d" % i, [128, 512], BF16)) for i in range(3)]
                rinv = S_(nc.sbuf_tensor("rinv", [128, 512], F32))
                km = S_(nc.sbuf_tensor("km", [128, 8], F32))
                kmb = S_(nc.sbuf_tensor("kmb", [128, 8], BF16))
                gm = S_(nc.sbuf_tensor("gm", [128, 64], F32))
                rank = S_(nc.sbuf_tensor("rank", [128, 64], F32))
                MRq = S_(nc.sbuf_tensor("MRq", [128, 64], BF16))
                gmask = S_(nc.sbuf_tensor("gmask", [128, 3, 64], F32))
                En = S_(nc.sbuf_tensor("En", [128, 8, 128], BF16))
                CB = S_(nc.sbuf_tensor("CB", [128, 4, 512], BF16))
                bcol = S_(nc.sbuf_tensor("bcol", [128, 8, 2, 16], F32))
                LF = S_(nc.sbuf_tensor("LF", [128, TT], F32))
                KK = S_(nc.sbuf_tensor("KK", [128, TT], BF16))
                tmpB = S_(nc.sbuf_tensor("tmpB", [128, TT], BF16))
                E1 = S_(nc.sbuf_tensor("E1", [128, TO], BF16))
                Vi = S_(nc.sbuf_tensor("Vi", [64, 32, 128], BF16))
                kdT = S_(nc.sbuf_tensor("kdT", [64, 32, 128], BF16))
                sqt = [S_(nc.sbuf_tensor("sqt%d" % i, [128, 512], BF16)) for i in range(2)]
                qe = S_(nc.sbuf_tensor("qe", [128, TO], BF16))
                sg = S_(nc.sbuf_tensor("sg", [128, TO], BF16))
                OT = S_(nc.sbuf_tensor("OT", [128, TO], F32))
                rsn = S_(nc.sbuf_tensor("rsn", [128, 512], F32))
                Smb = S_(nc.sbuf_tensor("Smb", [128, 16, 128], BF16))
                Rst = S_(nc.sbuf_tensor("Rst", [128, 128], F32))
                T1 = [S_(nc.sbuf_tensor("T1_%d" % i, [128, 128], F32)) for i in range(2)]
                ATs = S_(nc.sbuf_tensor("ATs", [64, 8, 64], BF16))
                m01 = S_(nc.sbuf_tensor("m01", [64, 64], F32))
                onesf = S_(nc.sbuf_tensor("onesf", [128, 512], F32))
                Bmc = S_(nc.sbuf_tensor("Bmc", [128, 32], F32))
                gcol = S_(nc.sbuf_tensor("gcol", [128, 32], F32))
                lbt = S_(nc.sbuf_tensor("lbt", [128, 16], F32))
                lbc = S_(nc.sbuf_tensor("lbc", [128, 8], F32))
                oml = S_(nc.sbuf_tensor("oml", [128, 8], F32))
                ong = S_(nc.sbuf_tensor("ong_sb", [128, 1], F32))
                iT = tmpB
                E2 = tmpB
                sqo = E1

                DMA("sp", gmask[:], c_gmask.rearrange("p (a b) -> p a b", a=3), writes=[("gmask",)], key="c1")
                DMA("sp", En[:], c_en.rearrange("p (a b) -> p a b", a=8), writes=[("En",)], key="c2")
                DMA("sp", CB[:], c_cb.rearrange("p (a b) -> p a b", a=4), writes=[("CB",)], key="c3")
                DMA("sp", bcol[:], c_bcol.rearrange("p (a b c) -> p a b c", a=8, b=2), writes=[("bcol",)], key="c4")
                DMA("sp", m01[:], c_m01, writes=[("m01",)], key="c5")
                DMA("sp", lbt[:], lbl, writes=[("lbt",)], key="c6")
                DMA("sp", ong[:], ong_d, writes=[("ong",)], key="c7")
                I("dve", "memset", onesf[:], 1.0, writes=[("onesf",)])
                I("dve", "memset", MRT[:], 0.0, writes=[("MRT",)])
                I("dve", "tensor_tensor", out=lbc[:], in0=lbt[:, 0:8], in1=lbt[:, 8:16], op=ALU.subtract, reads=[("lbt",)], writes=[("lbc",)])
                I("act", "activation", out=lbc[:], in_=lbc[:], func=AF.Sigmoid, reads=[("lbc",)], writes=[("lbc",)])
                I("dve", "tensor_scalar", out=oml[:], in0=lbc[:], scalar1=-1.0, scalar2=1.0, op0=ALU.mult, op1=ALU.add,
                  reads=[("lbc",)], writes=[("oml",)])

                wslot = [0]

                def load_w(col0):
                    s = wslot[0] % NWP
                    wslot[0] += 1
                    DMA("pool", Wp[s][:], w_in[:, col0:col0 + 128].rearrange("(c p) n -> p c n", p=128), writes=[("Wp", s)], key=("Wp", s))
                    return s

                def proj_fm(s, tg, evac):
                    pi = next_psA()
                    for c in range(NCH):
                        I("pe", "matmul", psA[pi][:], lhsT=Wp[s][:, c, :], rhs=hT[:, c, tg * 512:(tg + 1) * 512],
                          start=(c == 0), stop=(c == NCH - 1),
                          reads=[("Wp", s)] + [("hT", tg * 4 + k) for k in range(4)], writes=[("psA", pi)])
                    evac(pi)

                bg = []

                def run_bg(n):
                    for _ in range(n):
                        if bg:
                            bg.pop(0)()

                import os as _os
                _sub = int(_os.environ.get("KSUB", "0"))

                _subh = int(_os.environ.get("KSUBH", "0"))
                cur_h = [0]

                def ck(n):
                    if _sub == n and cur_h[0] == _subh:
                        for _k in range(int(_os.environ.get("KDUMPE", "0"))):
                            I("pe", "matmul", psH[:, 2, :], lhsT=kdT[:, 6, :], rhs=Vi[:, 6, :], start=True, stop=True,
                              reads=[("kdT", 0), ("Vi", 0)], writes=[("psH", 2)])
                        for _k in range(int(_os.environ.get("KDUM", "0"))):
                            I("dve", "memset", rank[:], 0.0, writes=[("rank",)])
                        DMA("sp", dbg["oT"], oT[:].rearrange("p a b -> p (a b)"), reads=[("oT",)], key="dbg1")
                        P.emit()
                        raise _Stop(nc)

                for h in range(nheads):
                    cur_h[0] = h
                    if h == 0:
                        pre = [load_w(IR0), load_w(FR0), load_w(QR0), load_w(GR0)]
                    sI, sF, sQ, sG = pre
                    for tg in range(4):
                        def ev_i(pi, tg=tg):
                            evac_copy(iT[:, tg * 512:(tg + 1) * 512], psA[pi][:], [("psA", pi)], [("tmpB", tg)])
                        proj_fm(sI, tg, ev_i)
                    for grp in range(4):
                        for k in range(8):
                            c = grp * 8 + k
                            I("pe", "transpose", out=psT[0:64, k, :], in_=iT[:, c * 64:(c + 1) * 64], identity=ident[:],
                              reads=[("tmpB", c // 8), ("ident",)], writes=[("psT", k)])
                        evac_copy(Vi[:, grp * 8:(grp + 1) * 8, :], psT[0:64, :, :], [("psT",)], [("Vi", grp)])
                    ck(1)
                    for tg in range(4):
                        def ev_f(pi, tg=tg):
                            I("act", "activation", out=LF[:, tg * 512:(tg + 1) * 512], in_=psA[pi][:], func=AF.Sigmoid,
                              reads=[("psA", pi)], writes=[("LF", tg)])
                            I("act", "activation", out=KK[:, tg * 512:(tg + 1) * 512], in_=psA[pi][:], func=AF.Sigmoid, scale=-1.0,
                              reads=[("psA", pi)], writes=[("KK", tg)])
                        proj_fm(sF, tg, ev_f)
                    LF3 = LF[:].rearrange("p (c s) -> p c s", s=64)
                    chain = []
                    chain.append(lambda h=h: I("dve", "tensor_scalar", out=LF[:], in0=LF[:], scalar1=oml[:, h:h + 1], scalar2=lbc[:, h:h + 1],
                                               op0=ALU.mult, op1=ALU.add, reads=[("LF",), ("oml",), ("lbc",)], writes=[("LF",)]))
                    chain.append(lambda: I("act", "activation", out=LF[:], in_=LF[:], func=AF.Ln, reads=[("LF",)], writes=[("LF",)]))
                    for k in range(4):
                        init = 0.0 if k == 0 else LF[:, k * 512 - 1:k * 512]
                        chain.append(lambda k=k, init=init: I("dve", "tensor_tensor_scan", out=LF[:, k * 512:(k + 1) * 512], data0=onesf[:],
                                                              data1=LF[:, k * 512:(k + 1) * 512], initial=init, op0=ALU.mult, op1=ALU.add,
                                                              reads=[("LF",), ("onesf",)], writes=[("LF",)]))

                    def _bm():
                        I("dve", "tensor_copy", out=Bmc[:], in_=LF3[:, :, 31], reads=[("LF",)], writes=[("Bmc",)])
                        I("dve", "tensor_tensor", out=gcol[:, 0:31], in0=Bmc[:, 1:32], in1=Bmc[:, 0:31], op=ALU.subtract,
                          reads=[("Bmc",)], writes=[("gcol",)])
                    chain.append(_bm)
                    chain.append(lambda: I("dve", "tensor_tensor", out=LF3, in0=LF3, in1=Bmc[:].unsqueeze(2).broadcast_to([128, 32, 64]),
                                           op=ALU.subtract, reads=[("LF",), ("Bmc",)], writes=[("LF",)]))
                    chain.append(lambda: I("act", "activation", out=gcol[:, 0:31], in_=gcol[:, 0:31], func=AF.Exp, reads=[("gcol",)], writes=[("gcol",)]))
                    chain.append(lambda: I("act", "activation", out=E2[:], in_=LF[:], func=AF.Exp, scale=-1.0, reads=[("LF",)], writes=[("tmpB",)]))
                    chain.append(lambda: I("act", "activation", out=E1[:], in_=LF[:, TO:TT], func=AF.Exp, reads=[("LF",)], writes=[("E1",)]))
                    chain.append(lambda h=h: I("dve", "scalar_tensor_tensor", out=KK[:], in0=KK[:], scalar=oml[:, h:h + 1], in1=E2[:],
                                               op0=ALU.mult, op1=ALU.mult, reads=[("KK",), ("tmpB",), ("oml",)], writes=[("KK",)]))

                    def after_group():
                        if chain:
                            chain.pop(0)()

                    for tgo in range(2):
                        def ev_q(pi, tgo=tgo):
                            I("act", "activation", out=sqt[tgo][:], in_=psA[pi][:], func=AF.Silu, reads=[("psA", pi)], writes=[("sqt", tgo)])
                            after_group()
                        proj_fm(sQ, 2 + tgo, ev_q)
                    for tgo in range(2):
                        def ev_g(pi, tgo=tgo):
                            I("act", "activation", out=sg[:, tgo * 512:(tgo + 1) * 512], in_=psA[pi][:], func=AF.Silu,
                              reads=[("psA", pi)], writes=[("sg", tgo)])
                            after_group()
                        proj_fm(sG, 2 + tgo, ev_g)
                    sK = load_w(KA0 + h * 128)
                    sQa = load_w(QA0 + h * 128)
                    sV = load_w(VA0 + h * 128)
                    DMA("sp", MRT[8:10, :], c_arow[h], writes=[("MRT", "a")], key="arow")
                    if h + 1 < nheads:
                        pre = [load_w(IR0 + (h + 1) * 128), load_w(FR0 + (h + 1) * 128)]
                    for tg in range(4):
                        def ev_k(pi, tg=tg):
                            evac_copy(KT[:, tg * 512:(tg + 1) * 512], psA[pi][:], [("psA", pi)], [("KT", tg)])
                            after_group()
                        proj_fm(sK, tg, ev_k)
                    for tgo in range(2):
                        def ev_qa(pi, tgo=tgo):
                            I("act", "mul", out=QT[:, tgo * 512:(tgo + 1) * 512], in_=psA[pi][:], mul=isq,
                              reads=[("psA", pi)], writes=[("QT", tgo)])
                            after_group()
                        proj_fm(sQa, 2 + tgo, ev_qa)
                    for g4 in range(4):
                        pi = next_psA()
                        for k in range(4):
                            i = g4 * 4 + k
                            for c in range(NCH):
                                I("pe", "matmul", psA[pi][:, k * 128:(k + 1) * 128], lhsT=hT[:, c, i * 128:(i + 1) * 128], rhs=Wp[sV][:, c, :],
                                  start=(c == 0), stop=(c == NCH - 1), reads=[("Wp", sV), ("hT", i)], writes=[("psA", pi)])
                        evac_copy(V[:, g4 * 4:(g4 + 1) * 4, :], psA[pi][:].rearrange("p (a b) -> p a b", a=4), [("psA", pi)], [("V", g4)])
                        after_group()
                    while chain:
                        after_group()
                    for tgo in range(2):
                        I("dve", "tensor_tensor", out=qe[:, tgo * 512:(tgo + 1) * 512], in0=sqt[tgo][:], in1=E1[:, tgo * 512:(tgo + 1) * 512],
                          op=ALU.mult, reads=[("sqt", tgo), ("E1",)], writes=[("qe", tgo)])
                    ck(2)
                    for grp in range(4):
                        for k in range(8):
                            c = grp * 8 + k
                            I("pe", "transpose", out=psT[0:64, k, :], in_=KK[:, c * 64:(c + 1) * 64], identity=ident[:],
                              reads=[("KK",), ("ident",)], writes=[("psT", k)])
                        evac_copy(kdT[:, grp * 8:(grp + 1) * 8, :], psT[0:64, :, :], [("psT",)], [("kdT", grp)])

                    def make_u(c):
                        def task():
                            ub, ubn = [(psH, ("psH",)), (psA[0], ("psA", 0)), (psA[1], ("psA", 1))][c % 3]
                            tb = c % 2
                            I("pe", "matmul", ub[:, 0:128], lhsT=kdT[:, c, :], rhs=Vi[:, c, :], start=True, stop=True,
                              reads=[("kdT", c // 8), ("Vi", c // 8)], writes=[ubn])
                            if c == 0:
                                I("dve", "tensor_copy", out=T1[tb][:], in_=ub[:, 0:128], reads=[ubn], writes=[("T1", tb)])
                            else:
                                I("dve", "tensor_tensor", out=T1[tb][:], in0=ub[:, 0:128], in1=Rst[:], op=ALU.add,
                                  reads=[ubn, ("Rst",)], writes=[("T1", tb)])
                            I("dve", "tensor_scalar", out=Rst[:], in0=T1[tb][:], scalar1=gcol[:, c:c + 1], scalar2=None, op0=ALU.mult,
                              reads=[("T1", tb), ("gcol",)], writes=[("Rst",)])
                            if c + 1 >= 16:
                                j = c + 1 - 16
                                I("pool", "tensor_scalar", out=Smb[:, j, :], in0=T1[tb][:], scalar1=gcol[:, c:c + 1], scalar2=None, op0=ALU.mult,
                                  reads=[("T1", tb), ("gcol",)], writes=[("Smb", j)])
                        return task
                    for c in range(31):
                        bg.append(make_u(c))
                    ck(3)
                    ck(4)
                    run_bg(4)
                    ck(5)
                    I("dve", "tensor_reduce", out=km[:], in_=KT[:].rearrange("p (n s) -> p n s", s=256), axis=AX.X, op=ALU.add,
                      reads=[("KT",)], writes=[("km",)])
                    I("dve", "tensor_scalar", out=kmb[:], in0=km[:], scalar1=1.0 / 256, scalar2=None, op0=ALU.mult,
                      reads=[("km",)], writes=[("kmb",)])
                    pg = next_psA()
                    for qt in range(8):
                        I("pe", "matmul", psA[pg][:, qt * 8:(qt + 1) * 8], lhsT=QT[:, qt * 128:(qt + 1) * 128], rhs=kmb[:], start=True, stop=True,
                          reads=[("QT", qt // 4), ("kmb",)], writes=[("psA", pg)])
                    I("dve", "tensor_tensor", out=gm[:], in0=psA[pg][:, 0:64], in1=gmask[:, 2, :], op=ALU.add,
                      reads=[("psA", pg), ("gmask",)], writes=[("gm",)])
                    ck(51)
                    gm3 = gm[:].rearrange("p (q m) -> p q m", m=8)
                    I("dve", "tensor_tensor", out=rinv[:].rearrange("p (q n m) -> p q n m", n=8, m=8),
                      in0=gm3.unsqueeze(2).broadcast_to([128, 8, 8, 8]), in1=gm3.unsqueeze(3).broadcast_to([128, 8, 8, 8]), op=ALU.is_gt,
                      reads=[("gm",)], writes=[("rinv",)])
                    I("dve", "tensor_reduce", out=rank[:], in_=rinv[:].rearrange("p (q m) -> p q m", m=8), axis=AX.X, op=ALU.add,
                      reads=[("rinv",)], writes=[("rank",)])
                    I("dve", "scalar_tensor_tensor", out=rank[:], in0=rank[:], scalar=3.0, in1=gmask[:, 0, :], op0=ALU.is_lt, op1=ALU.mult,
                      reads=[("rank",), ("gmask",)], writes=[("rank",)])
                    I("dve", "tensor_tensor", out=rank[:], in0=rank[:], in1=gmask[:, 1, :], op=ALU.add,
                      reads=[("rank",), ("gmask",)], writes=[("rank",)])
                    I("dve", "tensor_scalar", out=MRq[:], in0=rank[:], scalar1=-1.0, scalar2=-NEG, op0=ALU.add, op1=ALU.mult,
                      reads=[("rank",)], writes=[("MRq",)])
                    ck(52)
                    for qt in range(8):
                        I("pe", "transpose", out=psT[0:8, qt, :], in_=MRq[:, qt * 8:(qt + 1) * 8], identity=ident[:],
                          reads=[("MRq",), ("ident",)], writes=[("psT", qt)])
                    ck(53)
                    evac_copy(MRT[0:8, :].rearrange("p (a b) -> p a b", a=8), psT[0:8, :, :], [("psT",)], [("MRT", "m")])
                    ck(54)
                    run_bg(1)
                    ck(55)
                    run_bg(1)
                    ck(56)
                    run_bg(1)
                    ck(57)
                    run_bg(1)

                    ck(6)
                    if h + 1 < nheads:
                        pre = pre + [load_w(QR0 + (h + 1) * 128), load_w(GR0 + (h + 1) * 128)]
                    steps = [(G, kt) for G in range(2) for kt in range(8 + 4 * G + 4)]

                    def emit_S(si):
                        G, kt = steps[si]
                        sl = si % 2
                        n = kt // 2
                        diag = kt >= 8 + 4 * G
                        I("pe", "matmul", psS[sl][:], lhsT=KT[:, kt * 128:(kt + 1) * 128], rhs=QT[:, G * 512:(G + 1) * 512], start=True, stop=False,
                          reads=[("KT", kt // 4), ("QT", G)], writes=[("psS", sl)])
                        I("pe", "matmul", psS[sl][:], lhsT=En[:, n, :], rhs=MRT[:, G * 512:(G + 1) * 512], start=False, stop=not diag,
                          reads=[("En",), ("MRT",)], writes=[("psS", sl)])
                        if diag:
                            j = kt - 8 - 4 * G
                            I("pe", "matmul", psS[sl][:], lhsT=ident[:], rhs=CB[:, j, :], start=False, stop=True,
                              reads=[("ident",), ("CB",)], writes=[("psS", sl)])
                        pb = si % 3
                        I("act", "activation", out=PT[pb][:], in_=psS[sl][:], func=AF.Exp, bias=bcol[:, h, G, kt:kt + 1],
                          reads=[("psS", sl), ("bcol",)], writes=[("PT", pb)])

                    def emit_PV(si):
                        G, kt = steps[si]
                        pb = si % 3
                        last = kt == 8 + 4 * G + 3
                        I("pe", "matmul", psO[:], lhsT=V[:, kt, :], rhs=PT[pb][:], start=(kt == 0), stop=last,
                          reads=[("V", kt // 4), ("PT", pb)], writes=[("psO",)])
                        I("pe", "matmul", psR[:], lhsT=ones_b[:], rhs=PT[pb][:], start=(kt == 0), stop=last,
                          reads=[("ones_b",), ("PT", pb)], writes=[("psR",)])
                        if last:
                            I("dve", "reciprocal", out=rinv[:], in_=psR[:], reads=[("psR",)], writes=[("rinv",)])
                            I("dve", "tensor_tensor", out=oT[:, h, G * 512:(G + 1) * 512], in0=psO[:], in1=rinv[:], op=ALU.mult,
                              reads=[("psO",), ("rinv",)], writes=[("oT", h, G)])

                    emit_S(0)
                    for si in range(len(steps)):
                        if si + 1 < len(steps):
                            emit_S(si + 1)
                        emit_PV(si)
                        run_bg(1)
                    run_bg(100)

                    ck(7)
                    def emit_AT(j):
                        c = 16 + j
                        sl = j % 8
                        ab = j % 2
                        I("pe", "matmul", psS[ab][0:64, 0:64], lhsT=KK[:, c * 64:(c + 1) * 64], rhs=qe[:, j * 64:(j + 1) * 64],
                          start=True, stop=True, reads=[("KK",), ("qe", j // 8)], writes=[("psS", ab)])
                        I("dve", "tensor_tensor", out=ATs[:, sl, :], in0=psS[ab][0:64, 0:64], in1=m01[:], op=ALU.mult,
                          reads=[("psS", ab), ("m01",)], writes=[("ATs", sl)])

                    def emit_o(j):
                        c = 16 + j
                        sl = j % 8
                        jj = j % 8
                        po = psO if j < 8 else psR
                        pon = "psO" if j < 8 else "psR"
                        I("pe", "matmul", po[:, jj * 64:(jj + 1) * 64], lhsT=Smb[:, j, :], rhs=qe[:, j * 64:(j + 1) * 64], start=True, stop=False,
                          reads=[("Smb", j), ("qe", j // 8)], writes=[(pon, jj)])
                        I("pe", "matmul", po[:, jj * 64:(jj + 1) * 64], lhsT=Vi[:, c, :], rhs=ATs[:, sl, :], start=False, stop=True,
                          reads=[("Vi", c // 8), ("ATs", sl)], writes=[(pon, jj)])
                        if jj == 7:
                            hh = j // 8
                            evac_copy(OT[:, hh * 512:(hh + 1) * 512], po[:], [(pon,)], [("OT", hh)])

                    emit_AT(0)
                    emit_AT(1)
                    for j in range(16):
                        if j + 2 < 16:
                            emit_AT(j + 2)
                        emit_o(j)
                    ck(8)
                    I("act", "activation", out=sqo[:], in_=OT[:], func=AF.Square, reads=[("OT",)], writes=[("E1",)])
                    for hh in range(2):
                        pi = next_psA()
                        I("pe", "matmul", psA[pi][:], lhsT=ones_b[:], rhs=sqo[:, hh * 512:(hh + 1) * 512], start=True, stop=True,
                          reads=[("ones_b",), ("E1",)], writes=[("psA", pi)])
                        I("act", "activation", out=rsn[:], in_=psA[pi][:], func=AF.Sqrt, bias=epsc[:], scale=1.0 / 128,
                          reads=[("psA", pi), ("epsc",)], writes=[("rsn",)])
                        I("dve", "reciprocal", out=rsn[:], in_=rsn[:], reads=[("rsn",)], writes=[("rsn",)])
                        I("dve", "scalar_tensor_tensor", out=OT[:, hh * 512:(hh + 1) * 512], in0=OT[:, hh * 512:(hh + 1) * 512],
                          scalar=ong[:, 0:1], in1=rsn[:], op0=ALU.mult, op1=ALU.mult,
                          reads=[("OT", hh), ("ong",), ("rsn",)], writes=[("OT", hh)])
                        I("dve", "tensor_tensor", out=oT[:, 8 + h, hh * 512:(hh + 1) * 512], in0=OT[:, hh * 512:(hh + 1) * 512],
                          in1=sg[:, hh * 512:(hh + 1) * 512], op=ALU.mult,
                          reads=[("OT", hh), ("sg", hh)], writes=[("oT", 8 + h, hh)])
                    P.maybe_barrier(int(_os.environ.get("KBAR", "900")))

                if debug:
                    DMA("sp", dbg["hT"], hT[:].rearrange("p a b -> p (a b)"), reads=[("hT",)], key="dbg0")
                    DMA("sp", dbg["oT"], oT[:].rearrange("p a b -> p (a b)"), reads=[("oT",)], key="dbg1")
                if upto == 2:
                    P.emit()
                    raise _Stop(nc)
                P.barrier()

        x1 = T_(nc.sbuf_tensor("x1", [128, 8, D], F32))
        h2T = T_(nc.sbuf_tensor("h2T", [128, NCH, TO], BF16))
        comb = T_(nc.sbuf_tensor("comb", [128, 8, 16], F32))
        gB = T_(nc.sbuf_tensor("gB3", [128, D], F32))
        with contextlib.ExitStack() as s3:
            S_ = s3.enter_context
            Wo = [S_(nc.sbuf_tensor("Wo%d" % i, [128, NCH, 512], BF16)) for i in range(2)]
            xr = [S_(nc.sbuf_tensor("xr%d" % i, [128, 512], F32)) for i in range(3)]
            hn2 = [S_(nc.sbuf_tensor("hn2_%d" % i, [128, D], BF16)) for i in range(2)]
            wrb = S_(nc.sbuf_tensor("wrb", [128, NCH, 20], BF16))
            brB = S_(nc.sbuf_tensor("brB", [128, 20], F32))
            lg = S_(nc.sbuf_tensor("lg", [128, 8, 20], F32))
            rt = {}
            for nm, shp in [("gmax", [128, 8]), ("gsh", [128, 8, 4]), ("gsum", [128, 8]), ("gone", [128, 8, 4]),
                            ("t16", [128, 8, 16]), ("els", [128, 8, 4]), ("els2", [128, 8, 4]), ("e1", [128, 8]),
                            ("e2", [128, 8]), ("m1", [128, 8, 4]), ("m2", [128, 8, 4]), ("r", [128, 8]),
                            ("w1", [128, 8]), ("w2", [128, 8]), ("ew", [128, 8, 4])]:
                rt[nm] = S_(nc.sbuf_tensor("r_" + nm, shp, F32))

            DMA("sp", gB[:], g_ffn.broadcast_to([128, D]), writes=[("gB",)], key="gB")
            DMA("pool", wrb[:], wr.rearrange("(c p) n -> p c n", p=128), writes=[("wrb",)], key="wrb")
            DMA("sp", brB[:], br.broadcast_to([128, 20]), writes=[("brB",)], key="brB")
            xrc = 0
            for db in range(4):
                ws = db % 2
                DMA("pool", Wo[ws][:], w_out[:, db * 512:(db + 1) * 512].rearrange("(c p) n -> p c n", p=128), writes=[("Wo", ws)], key=("Wo", ws))
                for i in range(8):
                    xs = xrc % 3
                    xrc += 1
                    DMA("sp", xr[xs][:], xc[TO + i * 128:TO + (i + 1) * 128, db * 512:(db + 1) * 512], writes=[("xr", xs)], key=("xr", xs))
                    pi = next_psA()
                    for c in range(NCH):
                        I("pe", "matmul", psA[pi][:], lhsT=oT[:, c, i * 128:(i + 1) * 128], rhs=Wo[ws][:, c, :], start=(c == 0), stop=(c == NCH - 1),
                          reads=[("oT",), ("Wo", ws)], writes=[("psA", pi)])
                    I("dve", "tensor_tensor", out=x1[:, i, db * 512:(db + 1) * 512], in0=psA[pi][:], in1=xr[xs][:], op=ALU.add,
                      reads=[("psA", pi), ("xr", xs)], writes=[("x1", i, db)])
            def p3_stage1(i):
                b_ = i % 2
                rms_tile(gB, x1[:, i, :], [("x1", i)], 16 + i, hn2[b_][:], ("hn2", b_), hn2[b_][:], ("hn2", b_))

            def p3_stage2(i):
                b_ = i % 2
                for half in range(2):
                    for c in range(8):
                        cc = half * 8 + c
                        I("pe", "transpose", out=psT[:, c, :], in_=hn2[b_][:, cc * 128:(cc + 1) * 128], identity=ident[:],
                          reads=[("hn2", b_), ("ident",)], writes=[("psT", c)])
                    evac_copy(h2T[:, half * 8:(half + 1) * 8, i * 128:(i + 1) * 128], psT[:], [("psT",)], [("h2T", i)])
                pi = next_psA()
                for c in range(NCH):
                    I("pe", "matmul", psA[pi][:, 0:20], lhsT=h2T[:, c, i * 128:(i + 1) * 128], rhs=wrb[:, c, :], start=(c == 0), stop=(c == NCH - 1),
                      reads=[("h2T", i), ("wrb",)], writes=[("psA", pi)])
                I("dve", "tensor_tensor", out=lg[:, i, :], in0=psA[pi][:, 0:20], in1=brB[:], op=ALU.add,
                  reads=[("psA", pi), ("brB",)], writes=[("lg", i)])

            p3_stage1(0)
            for i in range(8):
                if i + 1 < 8:
                    p3_stage1(i + 1)
                p3_stage2(i)
            gl = lg[:, :, 0:4]
            el = lg[:, :, 4:20].rearrange("p q (g e) -> p q g e", g=4)

            def RT(*names):
                return [(n,) for n in names]
            b84 = lambda ap: ap.unsqueeze(2).broadcast_to([128, 8, 4])
            I("dve", "tensor_reduce", out=rt["gmax"][:], in_=gl, axis=AX.X, op=ALU.max, reads=RT("lg"), writes=RT("gmax"))
            I("dve", "tensor_tensor", out=rt["gsh"][:], in0=gl, in1=b84(rt["gmax"][:]), op=ALU.subtract, reads=RT("lg", "gmax"), writes=RT("gsh"))
            I("dve", "tensor_scalar", out=rt["gone"][:], in0=rt["gsh"][:], scalar1=0.0, scalar2=None, op0=ALU.is_ge, reads=RT("gsh"), writes=RT("gone"))
            I("act", "activation", out=rt["gsh"][:], in_=rt["gsh"][:], func=AF.Exp, reads=RT("gsh"), writes=RT("gsh"))
            I("dve", "tensor_reduce", out=rt["gsum"][:], in_=rt["gsh"][:], axis=AX.X, op=ALU.add, reads=RT("gsh"), writes=RT("gsum"))
            I("dve", "reciprocal", out=rt["gsum"][:], in_=rt["gsum"][:], reads=RT("gsum"), writes=RT("gsum"))
            I("dve", "tensor_tensor", out=rt["t16"][:].rearrange("p q (g e) -> p q g e", g=4), in0=el,
              in1=rt["gone"][:].unsqueeze(3).broadcast_to([128, 8, 4, 4]), op=ALU.mult, reads=RT("lg", "gone"), writes=RT("t16"))
            I("dve", "tensor_reduce", out=rt["els"][:], in_=rt["t16"][:].rearrange("p q (g e) -> p q e g", g=4), axis=AX.X, op=ALU.add,
              reads=RT("t16"), writes=RT("els"))
            I("dve", "tensor_reduce", out=rt["e1"][:], in_=rt["els"][:], axis=AX.X, op=ALU.max, reads=RT("els"), writes=RT("e1"))
            I("dve", "tensor_tensor", out=rt["m1"][:], in0=rt["els"][:], in1=b84(rt["e1"][:]), op=ALU.is_ge, reads=RT("els", "e1"), writes=RT("m1"))
            I("dve", "scalar_tensor_tensor", out=rt["els2"][:], in0=rt["m1"][:], scalar=-1e30, in1=rt["els"][:], op0=ALU.mult, op1=ALU.add,
              reads=RT("m1", "els"), writes=RT("els2"))
            I("dve", "tensor_reduce", out=rt["e2"][:], in_=rt["els2"][:], axis=AX.X, op=ALU.max, reads=RT("els2"), writes=RT("e2"))
            I("dve", "tensor_tensor", out=rt["m2"][:], in0=rt["els2"][:], in1=b84(rt["e2"][:]), op=ALU.is_ge, reads=RT("els2", "e2"), writes=RT("m2"))
            I("dve", "tensor_tensor", out=rt["r"][:], in0=rt["e2"][:], in1=rt["e1"][:], op=ALU.subtract, reads=RT("e1", "e2"), writes=RT("r"))
            I("act", "activation", out=rt["r"][:], in_=rt["r"][:], func=AF.Exp, reads=RT("r"), writes=RT("r"))
            I("dve", "tensor_scalar", out=rt["w1"][:], in0=rt["r"][:], scalar1=1.0, scalar2=None, op0=ALU.add, reads=RT("r"), writes=RT("w1"))
            I("dve", "reciprocal", out=rt["w1"][:], in_=rt["w1"][:], reads=RT("w1"), writes=RT("w1"))
            I("dve", "tensor_tensor", out=rt["w1"][:], in0=rt["w1"][:], in1=rt["gsum"][:], op=ALU.mult, reads=RT("w1", "gsum"), writes=RT("w1"))
            I("dve", "tensor_tensor", out=rt["w2"][:], in0=rt["w1"][:], in1=rt["r"][:], op=ALU.mult, reads=RT("w1", "r"), writes=RT("w2"))
            I("dve", "tensor_tensor", out=rt["m1"][:], in0=rt["m1"][:], in1=b84(rt["w1"][:]), op=ALU.mult, reads=RT("m1", "w1"), writes=RT("m1"))
            I("dve", "tensor_tensor", out=rt["m2"][:], in0=rt["m2"][:], in1=b84(rt["w2"][:]), op=ALU.mult, reads=RT("m2", "w2"), writes=RT("m2"))
            I("dve", "tensor_tensor", out=rt["ew"][:], in0=rt["m1"][:], in1=rt["m2"][:], op=ALU.add, reads=RT("m1", "m2"), writes=RT("ew"))
            I("dve", "tensor_tensor", out=comb[:].rearrange("p q (g e) -> p q g e", g=4),
              in0=rt["gone"][:].unsqueeze(3).broadcast_to([128, 8, 4, 4]), in1=rt["ew"][:].unsqueeze(2).broadcast_to([128, 8, 4, 4]),
              op=ALU.mult, reads=RT("gone", "ew"), writes=RT("comb"))
            if debug:
                DMA("sp", dbg["x1"], x1[:].rearrange("p a b -> p (a b)"), reads=[("x1",)], key="dbg2")
                DMA("sp", dbg["comb"], comb[:].rearrange("p a b -> p (a b)"), reads=[("comb",)], key="dbg3")
            if upto == 3:
                P.emit()
                raise _Stop(nc)
            P.barrier()

        with contextlib.ExitStack() as s4:
            S_ = s4.enter_context
            Wgu = [S_(nc.sbuf_tensor("Wgu%d" % i, [128, NCH, 256], BF16)) for i in range(4)]
            Wd = [S_(nc.sbuf_tensor("Wd%d" % i, [128, 8, 512], BF16)) for i in range(2)]
            hidT = oT[:, 0:8, :]
            sgt = [oT[:, 8, 0:512], oT[:, 8, 512:1024]]
            junk = oT[:, 10:12, :].rearrange("p a b -> p (a b)")
            DMA("sp", gB[:], g_fin.broadcast_to([128, D]), writes=[("gB",)], key="gB")
            gus = 0
            dsl = 0
            psGU = [psA[0], psA[1], psS[0], psS[1]]
            psGUn = [("psA", 0), ("psA", 1), ("psS", 0), ("psS", 1)]
            psY = [psO, psR]
            psYn = [("psO",), ("psR",)]
            guc = 0
            yc = 0
            sgc = 0
            for ex in range(NE):
                for fq in range(4):
                    sg_ = gus % 4
                    su_ = (gus + 1) % 4
                    gus += 2
                    DMA("pool", Wgu[sg_][:], w_gate[ex, :, fq * 256:(fq + 1) * 256].rearrange("(c p) n -> p c n", p=128),
                        writes=[("Wgu", sg_)], key=("Wgu", sg_))
                    DMA("pool", Wgu[su_][:], w_up[ex, :, fq * 256:(fq + 1) * 256].rearrange("(c p) n -> p c n", p=128),
                        writes=[("Wgu", su_)], key=("Wgu", su_))
                    for fs in range(2):
                        fb = fq * 2 + fs
                        for tg in range(2):
                            ig = guc % 4
                            iu = (guc + 1) % 4
                            guc += 2
                            for c in range(NCH):
                                I("pe", "matmul", psGU[ig][:], lhsT=Wgu[sg_][:, c, fs * 128:(fs + 1) * 128], rhs=h2T[:, c, tg * 512:(tg + 1) * 512],
                                  start=(c == 0), stop=(c == NCH - 1), reads=[("Wgu", sg_), ("h2T",)], writes=[psGUn[ig]])
                            for c in range(NCH):
                                I("pe", "matmul", psGU[iu][:], lhsT=Wgu[su_][:, c, fs * 128:(fs + 1) * 128], rhs=h2T[:, c, tg * 512:(tg + 1) * 512],
                                  start=(c == 0), stop=(c == NCH - 1), reads=[("Wgu", su_), ("h2T",)], writes=[psGUn[iu]])
                            sb = sgc % 2
                            sgc += 1
                            I("act", "activation", out=sgt[sb], in_=psGU[ig][:], func=AF.Silu, reads=[psGUn[ig]], writes=[("sgt", sb)])
                            I("dve", "tensor_tensor", out=hidT[:, fb, tg * 512:(tg + 1) * 512], in0=psGU[iu][:], in1=sgt[sb], op=ALU.mult,
                              reads=[psGUn[iu], ("sgt", sb)], writes=[("hidT", fb, tg)])
                for db in range(4):
                    ds_ = dsl % 2
                    dsl += 1
                    DMA("pool", Wd[ds_][:], w_down[ex, :, db * 512:(db + 1) * 512].rearrange("(c p) n -> p c n", p=128),
                        writes=[("Wd", ds_)], key=("Wd", ds_))
                    for i in range(8):
                        iy = yc % 2
                        yc += 1
                        for fb in range(8):
                            I("pe", "matmul", psY[iy][:], lhsT=hidT[:, fb, i * 128:(i + 1) * 128], rhs=Wd[ds_][:, fb, :], start=(fb == 0), stop=(fb == 7),
                              reads=[("hidT", fb, i // 4), ("Wd", ds_)], writes=[psYn[iy]])
                        I("dve", "scalar_tensor_tensor", out=x1[:, i, db * 512:(db + 1) * 512], in0=psY[iy][:], scalar=comb[:, i, ex:ex + 1],
                          in1=x1[:, i, db * 512:(db + 1) * 512], op0=ALU.mult, op1=ALU.add,
                          reads=[psYn[iy], ("comb",), ("x1", i, db)], writes=[("x1", i, db)])
                P.maybe_barrier(900)
            for i in range(8):
                I("act", "activation", out=junk, in_=x1[:, i, :], func=AF.Square, accum_out=ss[:, 24 + i:25 + i],
                  reads=[("x1", i)], writes=[("junk",), ("ss", 24 + i)])
                I("act", "activation", out=rstd[:, 24 + i:25 + i], in_=ss[:, 24 + i:25 + i], func=AF.Sqrt, bias=epsc[:], scale=1.0 / D,
                  reads=[("ss", 24 + i), ("epsc",)], writes=[("rstd", 24 + i)])
                I("dve", "reciprocal", out=rstd[:, 24 + i:25 + i], in_=rstd[:, 24 + i:25 + i], reads=[("rstd", 24 + i)], writes=[("rstd", 24 + i)])
                I("dve", "scalar_tensor_tensor", out=x1[:, i, :], in0=x1[:, i, :], scalar=rstd[:, 24 + i:25 + i], in1=gB[:],
                  op0=ALU.mult, op1=ALU.mult, reads=[("x1", i), ("rstd", 24 + i), ("gB",)], writes=[("x1", i)])
                DMA("sp", y_out[i * 128:(i + 1) * 128, :], x1[:, i, :], reads=[("x1", i)], key=("yo", i % 2))
            P.emit()
    return nc


def _consts(half):
    bf = ml_dtypes.bfloat16
    slopes = 2.0 ** (-8.0 * np.arange(1, NH + 1) / NH)
    c = {}
    c["c_ident"] = np.eye(128, dtype=np.float32).astype(bf)
    gmk = np.zeros((3, 8, 8), np.float32)
    nmin = 0 if half == 1 else 4
    for qt in range(8):
        qb = 4 + qt // 2
        for n in range(8):
            past = (n < qb) and (n >= nmin)
            gmk[0, qt, n] = 1.0 if past else 0.0
            gmk[1, qt, n] = 1.0 if n == qb else 0.0
            gmk[2, qt, n] = 0.0 if past else -1e30
    c["c_gmask"] = np.ascontiguousarray(np.broadcast_to(gmk.reshape(1, 192), (128, 192))).astype(np.float32)
    en = np.zeros((128, 8, 128), np.float32)
    for n in range(8):
        en[n, n, :] = 1.0
        en[8, n, :] = 1.0
        en[9, n, :] = 1.0
    c["c_en"] = en.reshape(128, 1024).astype(bf)
    cb = np.zeros((128, 4, 512), np.float32)
    cc = np.arange(128)[:, None]
    tt = np.arange(512)[None, :]
    for j in range(4):
        cb[:, j, :] = np.where(tt >= 128 * j + cc, 0.0, NEG)
    c["c_cb"] = cb.reshape(128, 2048).astype(bf)
    bc = np.zeros((128, 8, 2, 16), np.float32)
    p = np.arange(128)
    for h in range(8):
        for G in range(2):
            for kt in range(16):
                bc[:, h, G, kt] = -slopes[h] * (1024 + 512 * G - (128 * kt + p))
    c["c_bcol"] = bc.reshape(128, 256).astype(np.float32)
    ar = np.zeros((8, 2, 1024), np.float32)
    trel = np.arange(1024) % 512
    for h in range(8):
        ar[h, 0] = -slopes[h] * (128 * (trel // 128))
        ar[h, 1] = -slopes[h] * (trel % 128)
    c["c_arow"] = ar.astype(bf)
    m01 = (np.arange(64)[:, None] <= np.arange(64)[None, :]).astype(np.float32)
    c["c_m01"] = m01
    return c


_NC_CACHE = {}


def kernel(x, norm_mix_g, w_in, hgrn_lb_logits, hgrn_out_norm_g, w_out, norm_ffn_g,
           w_group_router, b_group_router, w_expert_router, b_expert_router,
           w_gate, w_up, w_down, final_norm_g, _debug=False, _upto=9, _nheads=NH, _ncores=8):
    f32 = np.float32
    x = np.asarray(x, f32)
    B = x.shape[0]
    w_in0 = np.ascontiguousarray(np.asarray(w_in, f32)[0])
    w_out0 = np.ascontiguousarray(np.asarray(w_out, f32)[0])
    wg = np.ascontiguousarray(np.asarray(w_gate, f32)[0])
    wu = np.ascontiguousarray(np.asarray(w_up, f32)[0])
    wd = np.ascontiguousarray(np.asarray(w_down, f32)[0])
    wgr = np.asarray(w_group_router, f32)[0]
    wer = np.asarray(w_expert_router, f32)[0]
    wr = np.ascontiguousarray(np.concatenate([wgr] + [wer[g] for g in range(4)], axis=1))
    br = np.ascontiguousarray(np.concatenate([np.asarray(b_group_router, f32)[0].reshape(-1),
                                              np.asarray(b_expert_router, f32)[0].reshape(-1)]).reshape(1, 20))
    lbl = np.asarray(hgrn_lb_logits, f32)
    lbl_t = np.ascontiguousarray(lbl.reshape(2, 8, 128).transpose(2, 0, 1).reshape(128, 16))
    ong = np.ascontiguousarray(np.asarray(hgrn_out_norm_g, f32)[0].reshape(128, 1))
    g_mix = np.ascontiguousarray(np.asarray(norm_mix_g, f32)[0].reshape(1, D))
    g_ffn = np.ascontiguousarray(np.asarray(norm_ffn_g, f32)[0].reshape(1, D))
    g_fin = np.ascontiguousarray(np.asarray(final_norm_g, f32).reshape(1, D))

    key = (bool(_debug), _upto, _nheads)
    if key not in _NC_CACHE:
        _NC_CACHE[key] = build_nc(debug=_debug, upto=_upto, nheads=_nheads)
    nc = _NC_CACHE[key]
    consts = [_consts(0), _consts(1)]
    in_maps = []
    for core in range(_ncores):
        b, half = core // 2, core % 2
        xcore = np.zeros((TT, D), f32)
        if half == 1:
            xcore[:TO] = x[b, :TO]
            xcore[TO:] = x[b, TO:]
        else:
            xcore[TO:] = x[b, :TO]
        m = dict(xc=xcore, w_in=w_in0, w_out=w_out0, w_gate=wg, w_up=wu, w_down=wd, wr=wr, br=br,
                 g_mix=g_mix, g_ffn=g_ffn, g_fin=g_fin, lbl=lbl_t, ong=ong)
        m.update(consts[half])
        in_maps.append(m)
    res = run_bass_kernel_spmd(nc, in_maps, core_ids=list(range(_ncores)))
    out = np.zeros((B, 2048, D), f32)
    for core in range(_ncores):
        b, half = core // 2, core % 2
        out[b, half * TO:(half + 1) * TO] = res.results[core]["y"]
    if _debug:
        return out, res.results
    return out
```

```python
import contextlib
import numpy as np
import ml_dtypes
import concourse.bass as bass
import concourse.mybir as mybir
from concourse.bass_utils import run_bass_kernel_spmd

F32 = mybir.dt.float32
BF16 = mybir.dt.bfloat16
AF = mybir.ActivationFunctionType
ALU = mybir.AluOpType
AX = mybir.AxisListType

ENGS = ("pe", "act", "dve", "pool", "sp")


class Op:
    __slots__ = ("eng", "fn", "reads", "writes", "dma", "key", "sig", "sigval",
                 "idx", "dmaval", "waits", "epoch")

    def __init__(self, eng, fn, reads, writes, dma, key, epoch):
        self.eng = eng
        self.fn = fn
        self.reads = reads
        self.writes = writes
        self.dma = dma
        self.key = key
        self.sig = False
        self.sigval = 0
        self.dmaval = 0
        self.waits = []
        self.epoch = epoch


class Prog:
    def __init__(self, nc, same_engine_sync=("act", "dve", "pool")):
        self.nc = nc
        self.ops = []
        self.same = set(same_engine_sync)
        self.state = {}
        self.children = {}
        self.dma_count = {}
        self.epoch = 0
        self.nsig = {e: 0 for e in ENGS}

    def _related(self, tok):
        out = []
        for i in range(1, len(tok) + 1):
            p = tok[:i]
            if p in self.state:
                out.append(p)
        for t in self.children.get(tok, ()):
            out.append(t)
        return out

    def _touch(self, tok):
        if tok not in self.state:
            self.state[tok] = [None, []]
            for i in range(1, len(tok)):
                self.children.setdefault(tok[:i], set()).add(tok)
        return self.state[tok]

    def add(self, eng, fn, reads=(), writes=(), dma=False, key=None):
        op = Op(eng, fn, [tuple(r) for r in reads], [tuple(w) for w in writes], dma, key, self.epoch)
        op.idx = len(self.ops)
        deps = {}
        for r in op.reads:
            for t in self._related(r):
                w = self.state[t][0]
                if w is not None:
                    deps[id(w)] = w
        for wtok in op.writes:
            for t in self._related(wtok):
                st = self.state[t]
                if st[0] is not None:
                    deps[id(st[0])] = st[0]
                for rd in st[1]:
                    deps[id(rd)] = rd
        for d in deps.values():
            self._dep(op, d)
        for r in op.reads:
            rl = self._touch(r)[1]
            if not dma:
                rl[:] = [o for o in rl if o.dma or o.eng != eng]
            rl.append(op)
        for wtok in op.writes:
            st = self._touch(wtok)
            st[0] = op
            st[1] = []
            for t in list(self.children.get(wtok, ())):
                self.state[t] = [op, []]
        if dma:
            self.dma_count[key] = self.dma_count.get(key, 0) + 16
            op.dmaval = self.dma_count[key]
        self.ops.append(op)
        return op

    def _dep(self, op, d):
        if d.dma:
            op.waits.append(("dma", d.key, self.dma_count[d.key]))
        else:
            if d.eng == op.eng and not op.dma and d.eng not in self.same:
                return
            if not d.sig:
                d.sig = True
                self.nsig[d.eng] += 1
            op.waits.append(("eng", d))

    def barrier(self):
        last_nd = {}
        for o in reversed(self.ops):
            if o.epoch != self.epoch:
                break
            if not o.dma and o.eng not in last_nd:
                last_nd[o.eng] = o
            if len(last_nd) == len(ENGS):
                break
        dkeys = dict(self.dma_count)
        for eng in ENGS:
            def nopfn(e):
                return e.nop()
            op = Op(eng, nopfn, [], [], False, None, self.epoch)
            op.idx = len(self.ops)
            for o in last_nd.values():
                if o.eng == eng and eng not in self.same:
                    continue
                if not o.sig:
                    o.sig = True
                op.waits.append(("eng", o))
            for k, v in dkeys.items():
                op.waits.append(("dma", k, v))
            self.ops.append(op)
        self.state = {}
        self.children = {}
        self.epoch += 1
        self.nsig = {e: 0 for e in ENGS}

    def maybe_barrier(self, limit):
        if max(self.nsig.values()) > limit:
            self.barrier()

    def emit(self):
        nc = self.nc
        cnt = {}
        for op in self.ops:
            if op.sig and not op.dma:
                k = (op.eng, op.epoch)
                cnt[k] = cnt.get(k, 0) + 1
                op.sigval = cnt[k]
        self.sigcounts = cnt
        keys = sorted(self.dma_count.keys(), key=str)
        with contextlib.ExitStack() as st:
            esem = {}
            for ep in range(self.epoch + 1):
                for e in ENGS:
                    if (e, ep) in cnt:
                        esem[(e, ep)] = st.enter_context(nc.semaphore("s_%s_%d" % (e, ep)))
            dsem = {k: st.enter_context(nc.semaphore("d_%d" % i)) for i, k in enumerate(keys)}
            self.nsems = len(esem) + len(dsem)
            block = st.enter_context(nc.Block())

            def make(engname):
                def body(e):
                    waited = {}
                    for op in self.ops:
                        if op.eng != engname:
                            continue
                        for w in op.waits:
                            if w[0] == "dma":
                                sem, val, kk = dsem[w[1]], w[2], ("d", w[1])
                            else:
                                kk = (w[1].eng, w[1].epoch)
                                sem, val = esem[kk], w[1].sigval
                            if waited.get(kk, 0) >= val:
                                continue
                            waited[kk] = val
                            e.wait_ge(sem, val)
                        ins = op.fn(e)
                        if op.dma:
                            ins.then_inc(dsem[op.key], 16)
                        elif op.sig:
                            ins.then_inc(esem[(op.eng, op.epoch)], 1)
                    if engname == "sp":
                        for k in keys:
                            e.wait_ge(dsem[k], self.dma_count[k])
                return body

            block.tensor(make("pe"))
            block.scalar(make("act"))
            block.vector(make("dve"))
            block.gpsimd(make("pool"))
            block.sync(make("sp"))


D = 2048
TT = 2048
TO = 1024
NCH = 16
NH = 8
DH = 128
INC = 7168
NE = 16
DE = 1024
EPS = 1e-6
NEG = -30000.0
QA0, KA0, VA0, QR0, FR0, IR0, GR0 = 0, 1024, 2048, 3072, 4096, 5120, 6144


class _Stop(Exception):
    pass


def build_nc(debug=False, upto=9, nheads=NH):
    try:
        return _build_nc(debug, upto, nheads)
    except _Stop as s:
        return s.args[0]


def _build_nc(debug, upto, nheads):
    nc = bass.Bass("TRN2", target_bir_lowering=False)

    def din(name, shape, dt=F32):
        return nc.dram_tensor(name, list(shape), dt, kind="ExternalInput").ap()

    xc = din("xc", [TT, D])
    w_in = din("w_in", [D, INC])
    w_out = din("w_out", [D, D])
    w_gate = din("w_gate", [NE, D, DE])
    w_up = din("w_up", [NE, D, DE])
    w_down = din("w_down", [NE, DE, D])
    wr = din("wr", [D, 20])
    br = din("br", [1, 20])
    g_mix = din("g_mix", [1, D])
    g_ffn = din("g_ffn", [1, D])
    g_fin = din("g_fin", [1, D])
    lbl = din("lbl", [128, 16])
    ong_d = din("ong", [128, 1])
    c_ident = din("c_ident", [128, 128], BF16)
    c_gmask = din("c_gmask", [128, 3 * 64])
    c_en = din("c_en", [128, 8 * 128], BF16)
    c_cb = din("c_cb", [128, 4 * 512], BF16)
    c_bcol = din("c_bcol", [128, 8 * 2 * 16])
    c_arow = din("c_arow", [8, 2, 1024], BF16)
    c_m01 = din("c_m01", [64, 64])
    y_out = nc.dram_tensor("y", [TO, D], F32, kind="ExternalOutput").ap()
    dbg = {}
    if debug:
        dbg["oT"] = nc.dram_tensor("dbg_oT", [128, 16 * TO], BF16, kind="ExternalOutput").ap()
        dbg["x1"] = nc.dram_tensor("dbg_x1", [128, 8 * D], F32, kind="ExternalOutput").ap()
        dbg["comb"] = nc.dram_tensor("dbg_comb", [128, 8 * 16], F32, kind="ExternalOutput").ap()
        dbg["hT"] = nc.dram_tensor("dbg_hT", [128, 16 * TT], BF16, kind="ExternalOutput").ap()

    P = Prog(nc)

    def I(eng, name, *args, reads=(), writes=(), dma=False, key=None, **kw):
        def fn(e, name=name, args=args, kw=kw):
            return getattr(e, name)(*args, **kw)
        return P.add(eng, fn, reads, writes, dma, key)

    def DMA(eng, out, in_, reads=(), writes=(), key=None):
        return I(eng, "dma_start", out=out, in_=in_, reads=reads, writes=writes, dma=True, key=key)

    isq = float(DH) ** -0.5

    with contextlib.ExitStack() as top:
        T_ = top.enter_context
        psA = [T_(nc.psum_tensor("psA%d" % i, [128, 512], F32)) for i in range(2)]
        psS = [T_(nc.psum_tensor("psS%d" % i, [128, 512], F32)) for i in range(2)]
        psO = T_(nc.psum_tensor("psO", [128, 512], F32))
        psR = T_(nc.psum_tensor("psR", [128, 512], F32))
        psT = T_(nc.psum_tensor("psT", [128, 8, 128], BF16))
        psH = T_(nc.psum_tensor("psH", [128, 512], F32))
        oT = T_(nc.sbuf_tensor("oT", [128, 16, TO], BF16))
        ident = T_(nc.sbuf_tensor("ident", [128, 128], BF16))
        ones_b = T_(nc.sbuf_tensor("ones_b", [128, 128], BF16))
        epsc = T_(nc.sbuf_tensor("epsc", [128, 1], F32))
        ss = T_(nc.sbuf_tensor("ss", [128, 32], F32))
        rstd = T_(nc.sbuf_tensor("rstd", [128, 32], F32))

        DMA("sp", ident[:], c_ident, writes=[("ident",)], key="c0")
        I("dve", "memset", ones_b[:], 1.0, writes=[("ones_b",)])
        I("dve", "memset", epsc[:], EPS, writes=[("epsc",)])

        acc_ctr = [0]

        def next_psA():
            i = acc_ctr[0] % 2
            acc_ctr[0] += 1
            return i

        cp_ctr = [0]

        def evac_copy(out, in_, reads, writes):
            i = cp_ctr[0] % 2
            cp_ctr[0] += 1
            if i == 0:
                I("act", "copy", out=out, in_=in_, reads=reads, writes=writes)
            else:
                I("dve", "tensor_copy", out=out, in_=in_, reads=reads, writes=writes)

        def rms_tile(gB, src_ap, srctoks, idx, hn_ap, hn_tok, junk_ap, junk_tok):
            I("act", "activation", out=junk_ap, in_=src_ap, func=AF.Square, accum_out=ss[:, idx:idx + 1],
              reads=srctoks, writes=[junk_tok, ("ss", idx)])
            I("act", "activation", out=rstd[:, idx:idx + 1], in_=ss[:, idx:idx + 1], func=AF.Sqrt, bias=epsc[:], scale=1.0 / D,
              reads=[("ss", idx), ("epsc",)], writes=[("rstd", idx)])
            I("dve", "reciprocal", out=rstd[:, idx:idx + 1], in_=rstd[:, idx:idx + 1], reads=[("rstd", idx)], writes=[("rstd", idx)])
            I("dve", "scalar_tensor_tensor", out=hn_ap, in0=src_ap, scalar=rstd[:, idx:idx + 1], in1=gB[:], op0=ALU.mult, op1=ALU.mult,
              reads=list(srctoks) + [("rstd", idx), ("gB",)], writes=[hn_tok])

        with contextlib.ExitStack() as sHT:
            hT = sHT.enter_context(nc.sbuf_tensor("hT", [128, NCH, TT], BF16))
            with contextlib.ExitStack() as s1:
                S_ = s1.enter_context
                gB = S_(nc.sbuf_tensor("gB1", [128, D], F32))
                xt = [S_(nc.sbuf_tensor("xt%d" % i, [128, D], F32)) for i in range(4)]
                hn = [S_(nc.sbuf_tensor("hn%d" % i, [128, D], BF16)) for i in range(4)]
                DMA("sp", gB[:], g_mix.broadcast_to([128, D]), writes=[("gB",)], key="gB")
                def p1_stage1(i):
                    b_ = i % 4
                    DMA("sp", xt[b_][:], xc[i * 128:(i + 1) * 128, :], writes=[("xt", b_)], key=("xt", b_))
                    rms_tile(gB, xt[b_][:], [("xt", b_)], i, hn[b_][:], ("hn", b_), hn[b_][:], ("hn", b_))

                def p1_stage2(i):
                    b_ = i % 4
                    for half in range(2):
                        for c in range(8):
                            cc = half * 8 + c
                            I("pe", "transpose", out=psT[:, c, :], in_=hn[b_][:, cc * 128:(cc + 1) * 128], identity=ident[:],
                              reads=[("hn", b_), ("ident",)], writes=[("psT", c)])
                        evac_copy(hT[:, half * 8:(half + 1) * 8, i * 128:(i + 1) * 128], psT[:], [("psT",)], [("hT", i)])

                p1_stage1(0)
                p1_stage1(1)
                for i in range(16):
                    if i + 2 < 16:
                        p1_stage1(i + 2)
                    p1_stage2(i)
                if upto == 1:
                    DMA("sp", dbg["hT"], hT[:].rearrange("p a b -> p (a b)"), reads=[("hT",)], key="dbg0")
                    P.emit()
                    raise _Stop(nc)
                P.barrier()

            with contextlib.ExitStack() as s2:
                S_ = s2.enter_context
                NWP = 6
                Wp = [S_(nc.sbuf_tensor("Wp%d" % i, [128, NCH, 128], BF16)) for i in range(NWP)]
                QT = S_(nc.sbuf_tensor("QT", [128, TO], BF16))
                KT = S_(nc.sbuf_tensor("KT", [128, TT], BF16))
                V = S_(nc.sbuf_tensor("V", [128, 16, 128], BF16))
                MRT = S_(nc.sbuf_tensor("MRT", [128, TO], BF16))
                PT = [S_(nc.sbuf_tensor("PT%d" % i, [128, 512], BF16)) for i in range(3)]
                rinv = S_(nc.sbuf_tensor("rinv", [128, 512], F32))
                km = S_(nc.sbuf_tensor("km", [128, 8], F32))
                kmb = S_(nc.sbuf_tensor("kmb", [128, 8], BF16))
                gm = S_(nc.sbuf_tensor("gm", [128, 64], F32))
                rank = S_(nc.sbuf_tensor("rank", [128, 64], F32))
                MRq = S_(nc.sbuf_tensor("MRq", [128, 64], BF16))
                gmask = S_(nc.sbuf_tensor("gmask", [128, 3, 64], F32))
                En = S_(nc.sbuf_tensor("En", [128, 8, 128], BF16))
                CB = S_(nc.sbuf_tensor("CB", [128, 4, 512], BF16))
                bcol = S_(nc.sbuf_tensor("bcol", [128, 8, 2, 16], F32))
                LF = S_(nc.sbuf_tensor("LF", [128, TT], F32))
                KK = S_(nc.sbuf_tensor("KK", [128, TT], BF16))
                tmpB = S_(nc.sbuf_tensor("tmpB", [128, TT], BF16))
                E1 = S_(nc.sbuf_tensor("E1", [128, TO], BF16))
                Vi = S_(nc.sbuf_tensor("Vi", [64, 32, 128], BF16))
                kdT = S_(nc.sbuf_tensor("kdT", [64, 32, 128], BF16))
                sqt = [S_(nc.sbuf_tensor("sqt%d" % i, [128, 512], BF16)) for i in range(2)]
                qe = S_(nc.sbuf_tensor("qe", [128, TO], BF16))
                sg = S_(nc.sbuf_tensor("sg", [128, TO], BF16))
                OT = S_(nc.sbuf_tensor("OT", [128, TO], F32))
                rsn = S_(nc.sbuf_tensor("rsn", [128, 512], F32))
                Smb = S_(nc.sbuf_tensor("Smb", [128, 16, 128], BF16))
                Rst = S_(nc.sbuf_tensor("Rst", [128, 128], F32))
                T1 = [S_(nc.sbuf_tensor("T1_%d" % i, [128, 128], F32)) for i in range(2)]
                ATs = S_(nc.sbuf_tensor("ATs", [64, 8, 64], BF16))
                m01 = S_(nc.sbuf_tensor("m01", [64, 64], F32))
                onesf = S_(nc.sbuf_tensor("onesf", [128, 512], F32))
                Bmc = S_(nc.sbuf_tensor("Bmc", [128, 32], F32))
                gcol = S_(nc.sbuf_tensor("gcol", [128, 32], F32))
                lbt = S_(nc.sbuf_tensor("lbt", [128, 16], F32))
                lbc = S_(nc.sbuf_tensor("lbc", [128, 8], F32))
                oml = S_(nc.sbuf_tensor("oml", [128, 8], F32))
                ong = S_(nc.sbuf_tensor("ong_sb", [128, 1], F32))
                iT = tmpB
                E2 = tmpB
                sqo = E1

                DMA("sp", gmask[:], c_gmask.rearrange("p (a b) -> p a b", a=3), writes=[("gmask",)], key="c1")
                DMA("sp", En[:], c_en.rearrange("p (a b) -> p a b", a=8), writes=[("En",)], key="c2")
                DMA("sp", CB[:], c_cb.rearrange("p (a b) -> p a b", a=4), writes=[("CB",)], key="c3")
                DMA("sp", bcol[:], c_bcol.rearrange("p (a b c) -> p a b c", a=8, b=2), writes=[("bcol",)], key="c4")
                DMA("sp", m01[:], c_m01, writes=[("m01",)], key="c5")
                DMA("sp", lbt[:], lbl, writes=[("lbt",)], key="c6")
                DMA("sp", ong[:], ong_d, writes=[("ong",)], key="c7")
                I("dve", "memset", onesf[:], 1.0, writes=[("onesf",)])
                I("dve", "memset", MRT[:], 0.0, writes=[("MRT",)])
                I("dve", "tensor_tensor", out=lbc[:], in0=lbt[:, 0:8], in1=lbt[:, 8:16], op=ALU.subtract, reads=[("lbt",)], writes=[("lbc",)])
                I("act", "activation", out=lbc[:], in_=lbc[:], func=AF.Sigmoid, reads=[("lbc",)], writes=[("lbc",)])
                I("dve", "tensor_scalar", out=oml[:], in0=lbc[:], scalar1=-1.0, scalar2=1.0, op0=ALU.mult, op1=ALU.add,
                  reads=[("lbc",)], writes=[("oml",)])

                wslot = [0]

                def load_w(col0):
                    s = wslot[0] % NWP
                    wslot[0] += 1
                    DMA("pool", Wp[s][:], w_in[:, col0:col0 + 128].rearrange("(c p) n -> p c n", p=128), writes=[("Wp", s)], key=("Wp", s))
                    return s

                def proj_fm(s, tg, evac):
                    pi = next_psA()
                    for c in range(NCH):
                        I("pe", "matmul", psA[pi][:], lhsT=Wp[s][:, c, :], rhs=hT[:, c, tg * 512:(tg + 1) * 512],
                          start=(c == 0), stop=(c == NCH - 1),
                          reads=[("Wp", s)] + [("hT", tg * 4 + k) for k in range(4)], writes=[("psA", pi)])
                    evac(pi)

                bg = []

                def run_bg(n):
                    for _ in range(n):
                        if bg:
                            bg.pop(0)()

                import os as _os
                _sub = int(_os.environ.get("KSUB", "0"))

                _subh = int(_os.environ.get("KSUBH", "0"))
                cur_h = [0]

                def ck(n):
                    if _sub == n and cur_h[0] == _subh:
                        for _k in range(int(_os.environ.get("KDUMPE", "0"))):
                            I("pe", "matmul", psH[:, 2, :], lhsT=kdT[:, 6, :], rhs=Vi[:, 6, :], start=True, stop=True,
                              reads=[("kdT", 0), ("Vi", 0)], writes=[("psH", 2)])
                        for _k in range(int(_os.environ.get("KDUM", "0"))):
                            I("dve", "memset", rank[:], 0.0, writes=[("rank",)])
                        DMA("sp", dbg["oT"], oT[:].rearrange("p a b -> p (a b)"), reads=[("oT",)], key="dbg1")
                        P.emit()
                        raise _Stop(nc)

                for h in range(nheads):
                    cur_h[0] = h
                    if h == 0:
                        pre = [load_w(IR0), load_w(FR0), load_w(QR0), load_w(GR0)]
                    sI, sF, sQ, sG = pre
                    for tg in range(4):
                        def ev_i(pi, tg=tg):
                            evac_copy(iT[:, tg * 512:(tg + 1) * 512], psA[pi][:], [("psA", pi)], [("tmpB", tg)])
                        proj_fm(sI, tg, ev_i)
                    for grp in range(4):
                        for k in range(8):
                            c = grp * 8 + k
                            I("pe", "transpose", out=psT[0:64, k, :], in_=iT[:, c * 64:(c + 1) * 64], identity=ident[:],
                              reads=[("tmpB", c // 8), ("ident",)], writes=[("psT", k)])
                        evac_copy(Vi[:, grp * 8:(grp + 1) * 8, :], psT[0:64, :, :], [("psT",)], [("Vi", grp)])
                    ck(1)
                    for tg in range(4):
                        def ev_f(pi, tg=tg):
                            I("act", "activation", out=LF[:, tg * 512:(tg + 1) * 512], in_=psA[pi][:], func=AF.Sigmoid,
                              reads=[("psA", pi)], writes=[("LF", tg)])
                            I("act", "activation", out=KK[:, tg * 512:(tg + 1) * 512], in_=psA[pi][:], func=AF.Sigmoid, scale=-1.0,
                              reads=[("psA", pi)], writes=[("KK", tg)])
                        proj_fm(sF, tg, ev_f)
                    LF3 = LF[:].rearrange("p (c s) -> p c s", s=64)
                    chain = []
                    chain.append(lambda h=h: I("dve", "tensor_scalar", out=LF[:], in0=LF[:], scalar1=oml[:, h:h + 1], scalar2=lbc[:, h:h + 1],
                                               op0=ALU.mult, op1=ALU.add, reads=[("LF",), ("oml",), ("lbc",)], writes=[("LF",)]))
                    chain.append(lambda: I("act", "activation", out=LF[:], in_=LF[:], func=AF.Ln, reads=[("LF",)], writes=[("LF",)]))
                    for k in range(4):
                        init = 0.0 if k == 0 else LF[:, k * 512 - 1:k * 512]
                        chain.append(lambda k=k, init=init: I("dve", "tensor_tensor_scan", out=LF[:, k * 512:(k + 1) * 512], data0=onesf[:],
                                                              data1=LF[:, k * 512:(k + 1) * 512], initial=init, op0=ALU.mult, op1=ALU.add,
                                                              reads=[("LF",), ("onesf",)], writes=[("LF",)]))

                    def _bm():
                        I("dve", "tensor_copy", out=Bmc[:], in_=LF3[:, :, 31], reads=[("LF",)], writes=[("Bmc",)])
                        I("dve", "tensor_tensor", out=gcol[:, 0:31], in0=Bmc[:, 1:32], in1=Bmc[:, 0:31], op=ALU.subtract,
                          reads=[("Bmc",)], writes=[("gcol",)])
                    chain.append(_bm)
                    chain.append(lambda: I("dve", "tensor_tensor", out=LF3, in0=LF3, in1=Bmc[:].unsqueeze(2).broadcast_to([128, 32, 64]),
                                           op=ALU.subtract, reads=[("LF",), ("Bmc",)], writes=[("LF",)]))
                    chain.append(lambda: I("act", "activation", out=gcol[:, 0:31], in_=gcol[:, 0:31], func=AF.Exp, reads=[("gcol",)], writes=[("gcol",)]))
                    chain.append(lambda: I("act", "activation", out=E2[:], in_=LF[:], func=AF.Exp, scale=-1.0, reads=[("LF",)], writes=[("tmpB",)]))
                    chain.append(lambda: I("act", "activation", out=E1[:], in_=LF[:, TO:TT], func=AF.Exp, reads=[("LF",)], writes=[("E1",)]))
                    chain.append(lambda h=h: I("dve", "scalar_tensor_tensor", out=KK[:], in0=KK[:], scalar=oml[:, h:h + 1], in1=E2[:],
                                               op0=ALU.mult, op1=ALU.mult, reads=[("KK",), ("tmpB",), ("oml",)], writes=[("KK",)]))

                    def after_group():
                        if chain:
                            chain.pop(0)()

                    for tgo in range(2):
                        def ev_q(pi, tgo=tgo):
                            I("act", "activation", out=sqt[tgo][:], in_=psA[pi][:], func=AF.Silu, reads=[("psA", pi)], writes=[("sqt", tgo)])
                            after_group()
                        proj_fm(sQ, 2 + tgo, ev_q)
                    for tgo in range(2):
                        def ev_g(pi, tgo=tgo):
                            I("act", "activation", out=sg[:, tgo * 512:(tgo + 1) * 512], in_=psA[pi][:], func=AF.Silu,
                              reads=[("psA", pi)], writes=[("sg", tgo)])
                            after_group()
                        proj_fm(sG, 2 + tgo, ev_g)
                    sK = load_w(KA0 + h * 128)
                    sQa = load_w(QA0 + h * 128)
                    sV = load_w(VA0 + h * 128)
                    DMA("sp", MRT[8:10, :], c_arow[h], writes=[("MRT", "a")], key="arow")
                    if h + 1 < nheads:
                        pre = [load_w(IR0 + (h + 1) * 128), load_w(FR0 + (h + 1) * 128)]
                    for tg in range(4):
                        def ev_k(pi, tg=tg):
                            evac_copy(KT[:, tg * 512:(tg + 1) * 512], psA[pi][:], [("psA", pi)], [("KT", tg)])
                            after_group()
                        proj_fm(sK, tg, ev_k)
                    for tgo in range(2):
                        def ev_qa(pi, tgo=tgo):
                            I("act", "mul", out=QT[:, tgo * 512:(tgo + 1) * 512], in_=psA[pi][:], mul=isq,
                              reads=[("psA", pi)], writes=[("QT", tgo)])
                            after_group()
                        proj_fm(sQa, 2 + tgo, ev_qa)
                    for g4 in range(4):
                        pi = next_psA()
                        for k in range(4):
                            i = g4 * 4 + k
                            for c in range(NCH):
                                I("pe", "matmul", psA[pi][:, k * 128:(k + 1) * 128], lhsT=hT[:, c, i * 128:(i + 1) * 128], rhs=Wp[sV][:, c, :],
                                  start=(c == 0), stop=(c == NCH - 1), reads=[("Wp", sV), ("hT", i)], writes=[("psA", pi)])
                        evac_copy(V[:, g4 * 4:(g4 + 1) * 4, :], psA[pi][:].rearrange("p (a b) -> p a b", a=4), [("psA", pi)], [("V", g4)])
                        after_group()
                    while chain:
                        after_group()
                    for tgo in range(2):
                        I("dve", "tensor_tensor", out=qe[:, tgo * 512:(tgo + 1) * 512], in0=sqt[tgo][:], in1=E1[:, tgo * 512:(tgo + 1) * 512],
                          op=ALU.mult, reads=[("sqt", tgo), ("E1",)], writes=[("qe", tgo)])
                    ck(2)
                    for grp in range(4):
                        for k in range(8):
                            c = grp * 8 + k
                            I("pe", "transpose", out=psT[0:64, k, :], in_=KK[:, c * 64:(c + 1) * 64], identity=ident[:],
                              reads=[("KK",), ("ident",)], writes=[("psT", k)])
                        evac_copy(kdT[:, grp * 8:(grp + 1) * 8, :], psT[0:64, :, :], [("psT",)], [("kdT", grp)])

                    def make_u(c):
                        def task():
                            ub, ubn = [(psH, ("psH",)), (psA[0], ("psA", 0)), (psA[1], ("psA", 1))][c % 3]
                            tb = c % 2
                            I("pe", "matmul", ub[:, 0:128], lhsT=kdT[:, c, :], rhs=Vi[:, c, :], start=True, stop=True,
                              reads=[("kdT", c // 8), ("Vi", c // 8)], writes=[ubn])
                            if c == 0:
                                I("dve", "tensor_copy", out=T1[tb][:], in_=ub[:, 0:128], reads=[ubn], writes=[("T1", tb)])
                            else:
                                I("dve", "tensor_tensor", out=T1[tb][:], in0=ub[:, 0:128], in1=Rst[:], op=ALU.add,
                                  reads=[ubn, ("Rst",)], writes=[("T1", tb)])
                            I("dve", "tensor_scalar", out=Rst[:], in0=T1[tb][:], scalar1=gcol[:, c:c + 1], scalar2=None, op0=ALU.mult,
                              reads=[("T1", tb), ("gcol",)], writes=[("Rst",)])
                            if c + 1 >= 16:
                                j = c + 1 - 16
                                I("pool", "tensor_scalar", out=Smb[:, j, :], in0=T1[tb][:], scalar1=gcol[:, c:c + 1], scalar2=None, op0=ALU.mult,
                                  reads=[("T1", tb), ("gcol",)], writes=[("Smb", j)])
                        return task
                    for c in range(31):
                        bg.append(make_u(c))
                    ck(3)
                    ck(4)
                    run_bg(4)
                    ck(5)
                    I("dve", "tensor_reduce", out=km[:], in_=KT[:].rearrange("p (n s) -> p n s", s=256), axis=AX.X, op=ALU.add,
                      reads=[("KT",)], writes=[("km",)])
                    I("dve", "tensor_scalar", out=kmb[:], in0=km[:], scalar1=1.0 / 256, scalar2=None, op0=ALU.mult,
                      reads=[("km",)], writes=[("kmb",)])
                    pg = next_psA()
                    for qt in range(8):
                        I("pe", "matmul", psA[pg][:, qt * 8:(qt + 1) * 8], lhsT=QT[:, qt * 128:(qt + 1) * 128], rhs=kmb[:], start=True, stop=True,
                          reads=[("QT", qt // 4), ("kmb",)], writes=[("psA", pg)])
                    I("dve", "tensor_tensor", out=gm[:], in0=psA[pg][:, 0:64], in1=gmask[:, 2, :], op=ALU.add,
                      reads=[("psA", pg), ("gmask",)], writes=[("gm",)])
                    ck(51)
                    gm3 = gm[:].rearrange("p (q m) -> p q m", m=8)
                    I("dve", "tensor_tensor", out=rinv[:].rearrange("p (q n m) -> p q n m", n=8, m=8),
                      in0=gm3.unsqueeze(2).broadcast_to([128, 8, 8, 8]), in1=gm3.unsqueeze(3).broadcast_to([128, 8, 8, 8]), op=ALU.is_gt,
                      reads=[("gm",)], writes=[("rinv",)])
                    I("dve", "tensor_reduce", out=rank[:], in_=rinv[:].rearrange("p (q m) -> p q m", m=8), axis=AX.X, op=ALU.add,
                      reads=[("rinv",)], writes=[("rank",)])
                    I("dve", "scalar_tensor_tensor", out=rank[:], in0=rank[:], scalar=3.0, in1=gmask[:, 0, :], op0=ALU.is_lt, op1=ALU.mult,
                      reads=[("rank",), ("gmask",)], writes=[("rank",)])
                    I("dve", "tensor_tensor", out=rank[:], in0=rank[:], in1=gmask[:, 1, :], op=ALU.add,
                      reads=[("rank",), ("gmask",)], writes=[("rank",)])
                    I("dve", "tensor_scalar", out=MRq[:], in0=rank[:], scalar1=-1.0, scalar2=-NEG, op0=ALU.add, op1=ALU.mult,
                      reads=[("rank",)], writes=[("MRq",)])
                    ck(52)
                    for qt in range(8):
                        I("pe", "transpose", out=psT[0:8, qt, :], in_=MRq[:, qt * 8:(qt + 1) * 8], identity=ident[:],
                          reads=[("MRq",), ("ident",)], writes=[("psT", qt)])
                    ck(53)
                    evac_copy(MRT[0:8, :].rearrange("p (a b) -> p a b", a=8), psT[0:8, :, :], [("psT",)], [("MRT", "m")])
                    ck(54)
                    run_bg(1)
                    ck(55)
                    run_bg(1)
                    ck(56)
                    run_bg(1)
                    ck(57)
                    run_bg(1)

                    ck(6)
                    if h + 1 < nheads:
                        pre = pre + [load_w(QR0 + (h + 1) * 128), load_w(GR0 + (h + 1) * 128)]
                    steps = [(G, kt) for G in range(2) for kt in range(8 + 4 * G + 4)]

                    def emit_S(si):
                        G, kt = steps[si]
                        sl = si % 2
                        n = kt // 2
                        diag = kt >= 8 + 4 * G
                        I("pe", "matmul", psS[sl][:], lhsT=KT[:, kt * 128:(kt + 1) * 128], rhs=QT[:, G * 512:(G + 1) * 512], start=True, stop=False,
                          reads=[("KT", kt // 4), ("QT", G)], writes=[("psS", sl)])
                        I("pe", "matmul", psS[sl][:], lhsT=En[:, n, :], rhs=MRT[:, G * 512:(G + 1) * 512], start=False, stop=not diag,
                          reads=[("En",), ("MRT",)], writes=[("psS", sl)])
                        if diag:
                            j = kt - 8 - 4 * G
                            I("pe", "matmul", psS[sl][:], lhsT=ident[:], rhs=CB[:, j, :], start=False, stop=True,
                              reads=[("ident",), ("CB",)], writes=[("psS", sl)])
                        pb = si % 3
                        I("act", "activation", out=PT[pb][:], in_=psS[sl][:], func=AF.Exp, bias=bcol[:, h, G, kt:kt + 1],
                          reads=[("psS", sl), ("bcol",)], writes=[("PT", pb)])

                    def emit_PV(si):
                        G, kt = steps[si]
                        pb = si % 3
                        last = kt == 8 + 4 * G + 3
                        I("pe", "matmul", psO[:], lhsT=V[:, kt, :], rhs=PT[pb][:], start=(kt == 0), stop=last,
                          reads=[("V", kt // 4), ("PT", pb)], writes=[("psO",)])
                        I("pe", "matmul", psR[:], lhsT=ones_b[:], rhs=PT[pb][:], start=(kt == 0), stop=last,
                          reads=[("ones_b",), ("PT", pb)], writes=[("psR",)])
                        if last:
                            I("dve", "reciprocal", out=rinv[:], in_=psR[:], reads=[("psR",)], writes=[("rinv",)])
                            I("dve", "tensor_tensor", out=oT[:, h, G * 512:(G + 1) * 512], in0=psO[:], in1=rinv[:], op=ALU.mult,
                              reads=[("psO",), ("rinv",)], writes=[("oT", h, G)])

                    emit_S(0)
                    for si in range(len(steps)):
                        if si + 1 < len(steps):
                            emit_S(si + 1)
                        emit_PV(si)
                        run_bg(1)
                    run_bg(100)

                    ck(7)
                    def emit_AT(j):
                        c = 16 + j
                        sl = j % 8
                        ab = j % 2
                        I("pe", "matmul", psS[ab][0:64, 0:64], lhsT=KK[:, c * 64:(c + 1) * 64], rhs=qe[:, j * 64:(j + 1) * 64],
                          start=True, stop=True, reads=[("KK",), ("qe", j // 8)], writes=[("psS", ab)])
                        I("dve", "tensor_tensor", out=ATs[:, sl, :], in0=psS[ab][0:64, 0:64], in1=m01[:], op=ALU.mult,
                          reads=[("psS", ab), ("m01",)], writes=[("ATs", sl)])

                    def emit_o(j):
                        c = 16 + j
                        sl = j % 8
                        jj = j % 8
                        po = psO if j < 8 else psR
                        pon = "psO" if j < 8 else "psR"
                        I("pe", "matmul", po[:, jj * 64:(jj + 1) * 64], lhsT=Smb[:, j, :], rhs=qe[:, j * 64:(j + 1) * 64], start=True, stop=False,
                          reads=[("Smb", j), ("qe", j // 8)], writes=[(pon, jj)])
                        I("pe", "matmul", po[:, jj * 64:(jj + 1) * 64], lhsT=Vi[:, c, :], rhs=ATs[:, sl, :], start=False, stop=True,
                          reads=[("Vi", c // 8), ("ATs", sl)], writes=[(pon, jj)])
                        if jj == 7:
                            hh = j // 8
                            evac_copy(OT[:, hh * 512:(hh + 1) * 512], po[:], [(pon,)], [("OT", hh)])

                    emit_AT(0)
                    emit_AT(1)
                    for j in range(16):
                        if j + 2 < 16:
                            emit_AT(j + 2)
                        emit_o(j)
                    ck(8)
                    I("act", "activation", out=sqo[:], in_=OT[:], func=AF.Square, reads=[("OT",)], writes=[("E1",)])
                    for hh in range(2):
                        pi = next_psA()
                        I("pe", "matmul", psA[pi][:], lhsT=ones_b[:], rhs=sqo[:, hh * 512:(hh + 1) * 512], start=True, stop=True,
                          reads=[("ones_b",), ("E1",)], writes=[("psA", pi)])
                        I("act", "activation", out=rsn[:], in_=psA[pi][:], func=AF.Sqrt, bias=epsc[:], scale=1.0 / 128,
                          reads=[("psA", pi), ("epsc",)], writes=[("rsn",)])
                        I("dve", "reciprocal", out=rsn[:], in_=rsn[:], reads=[("rsn",)], writes=[("rsn",)])
                        I("dve", "scalar_tensor_tensor", out=OT[:, hh * 512:(hh + 1) * 512], in0=OT[:, hh * 512:(hh + 1) * 512],
                          scalar=ong[:, 0:1], in1=rsn[:], op0=ALU.mult, op1=ALU.mult,
                          reads=[("OT", hh), ("ong",), ("rsn",)], writes=[("OT", hh)])
                        I("dve", "tensor_tensor", out=oT[:, 8 + h, hh * 512:(hh + 1) * 512], in0=OT[:, hh * 512:(hh + 1) * 512],
                          in1=sg[:, hh * 512:(hh + 1) * 512], op=ALU.mult,
                          reads=[("OT", hh), ("sg", hh)], writes=[("oT", 8 + h, hh)])
                    P.maybe_barrier(int(_os.environ.get("KBAR", "900")))

                if debug:
                    DMA("sp", dbg["hT"], hT[:].rearrange("p a b -> p (a b)"), reads=[("hT",)], key="dbg0")
                    DMA("sp", dbg["oT"], oT[:].rearrange("p a b -> p (a b)"), reads=[("oT",)], key="dbg1")
                if upto == 2:
                    P.emit()
                    raise _Stop(nc)
                P.barrier()

        x1 = T_(nc.sbuf_tensor("x1", [128, 8, D], F32))
        h2T = T_(nc.sbuf_tensor("h2T", [128, NCH, TO], BF16))
        comb = T_(nc.sbuf_tensor("comb", [128, 8, 16], F32))
        gB = T_(nc.sbuf_tensor("gB3", [128, D], F32))
        lg = T_(nc.sbuf_tensor("lg", [128, 8, 20], F32))
        rt = {}
        for nm, shp in [("gmax", [128, 8]), ("gsh", [128, 8, 4]), ("gsum", [128, 8]), ("gone", [128, 8, 4]),
                        ("t16", [128, 8, 16]), ("els", [128, 8, 4]), ("els2", [128, 8, 4]), ("e1", [128, 8]),
                        ("e2", [128, 8]), ("m1", [128, 8, 4]), ("m2", [128, 8, 4]), ("r", [128, 8]),
                        ("w1", [128, 8]), ("w2", [128, 8]), ("ew", [128, 8, 4])]:
            rt[nm] = T_(nc.sbuf_tensor("r_" + nm, shp, F32))

        with contextlib.ExitStack() as s3:
            S_ = s3.enter_context
            Wo = [S_(nc.sbuf_tensor("Wo%d" % i, [128, NCH, 512], BF16)) for i in range(2)]
            xr = [S_(nc.sbuf_tensor("xr%d" % i, [128, 512], F32)) for i in range(3)]
            hn2 = [S_(nc.sbuf_tensor("hn2_%d" % i, [128, D], BF16)) for i in range(2)]
            wrb = S_(nc.sbuf_tensor("wrb", [128, NCH, 20], BF16))
            brB = S_(nc.sbuf_tensor("brB", [128, 20], F32))
            DMA("sp", gB[:], g_ffn.broadcast_to([128, D]), writes=[("gB",)], key="gB")
            DMA("pool", wrb[:], wr.rearrange("(c p) n -> p c n", p=128), writes=[("wrb",)], key="wrb")
            DMA("sp", brB[:], br.broadcast_to([128, 20]), writes=[("brB",)], key="brB")
            xrc = 0
            for db in range(4):
                ws = db % 2
                DMA("pool", Wo[ws][:], w_out[:, db * 512:(db + 1) * 512].rearrange("(c p) n -> p c n", p=128), writes=[("Wo", ws)], key=("Wo", ws))
                for i in range(8):
                    xs = xrc % 3
                    xrc += 1
                    DMA("sp", xr[xs][:], xc[TO + i * 128:TO + (i + 1) * 128, db * 512:(db + 1) * 512], writes=[("xr", xs)], key=("xr", xs))
                    pi = next_psA()
                    for c in range(NCH):
                        I("pe", "matmul", psA[pi][:], lhsT=oT[:, c, i * 128:(i + 1) * 128], rhs=Wo[ws][:, c, :], start=(c == 0), stop=(c == NCH - 1),
                          reads=[("oT",), ("Wo", ws)], writes=[("psA", pi)])
                    I("dve", "tensor_tensor", out=x1[:, i, db * 512:(db + 1) * 512], in0=psA[pi][:], in1=xr[xs][:], op=ALU.add,
                      reads=[("psA", pi), ("xr", xs)], writes=[("x1", i, db)])
            def p3_stage1(i):
                b_ = i % 2
                rms_tile(gB, x1[:, i, :], [("x1", i)], 16 + i, hn2[b_][:], ("hn2", b_), hn2[b_][:], ("hn2", b_))

            def p3_stage2(i):
                b_ = i % 2
                for half in range(2):
                    for c in range(8):
                        cc = half * 8 + c
                        I("pe", "transpose", out=psT[:, c, :], in_=hn2[b_][:, cc * 128:(cc + 1) * 128], identity=ident[:],
                          reads=[("hn2", b_), ("ident",)], writes=[("psT", c)])
                    evac_copy(h2T[:, half * 8:(half + 1) * 8, i * 128:(i + 1) * 128], psT[:], [("psT",)], [("h2T", i)])
                pi = next_psA()
                for c in range(NCH):
                    I("pe", "matmul", psA[pi][:, 0:20], lhsT=h2T[:, c, i * 128:(i + 1) * 128], rhs=wrb[:, c, :], start=(c == 0), stop=(c == NCH - 1),
                      reads=[("h2T", i), ("wrb",)], writes=[("psA", pi)])
                I("dve", "tensor_tensor", out=lg[:, i, :], in0=psA[pi][:, 0:20], in1=brB[:], op=ALU.add,
                  reads=[("psA", pi), ("brB",)], writes=[("lg", i)])

            p3_stage1(0)
            for i in range(8):
                if i + 1 < 8:
                    p3_stage1(i + 1)
                p3_stage2(i)
            if debug:
                DMA("sp", dbg["x1"], x1[:].rearrange("p a b -> p (a b)"), reads=[("x1",)], key="dbg2")
            if upto == 3:
                P.emit()
                raise _Stop(nc)
            P.barrier()

        with contextlib.ExitStack() as s4:
            S_ = s4.enter_context
            Wgu = [S_(nc.sbuf_tensor("Wgu%d" % i, [128, NCH, 256], BF16)) for i in range(4)]
            Wd = [S_(nc.sbuf_tensor("Wd%d" % i, [128, 8, 512], BF16)) for i in range(2)]
            hidT = oT[:, 0:8, :]
            sgt = [oT[:, 8, 0:512], oT[:, 8, 512:1024]]
            junk = oT[:, 10:12, :].rearrange("p a b -> p (a b)")
            DMA("sp", gB[:], g_fin.broadcast_to([128, D]), writes=[("gB",)], key="gB")
            gus = 0
            dsl = 0
            psGU = [psA[0], psA[1], psS[0], psS[1]]
            psGUn = [("psA", 0), ("psA", 1), ("psS", 0), ("psS", 1)]
            psY = [psO, psR]
            psYn = [("psO",), ("psR",)]
            guc = 0
            yc = 0
            sgc = 0
            for ex in range(NE):
                for fq in range(4):
                    sg_ = gus % 4
                    su_ = (gus + 1) % 4
                    gus += 2
                    DMA("pool", Wgu[sg_][:], w_gate[ex, :, fq * 256:(fq + 1) * 256].rearrange("(c p) n -> p c n", p=128),
                        writes=[("Wgu", sg_)], key=("Wgu", sg_))
                    DMA("pool", Wgu[su_][:], w_up[ex, :, fq * 256:(fq + 1) * 256].rearrange("(c p) n -> p c n", p=128),
                        writes=[("Wgu", su_)], key=("Wgu", su_))
                    if ex == 0 and fq == 0:
                        gl = lg[:, :, 0:4]
                        el = lg[:, :, 4:20].rearrange("p q (g e) -> p q g e", g=4)

                        def RT(*names):
                            return [(n,) for n in names]
                        b84 = lambda ap: ap.unsqueeze(2).broadcast_to([128, 8, 4])
                        I("dve", "tensor_reduce", out=rt["gmax"][:], in_=gl, axis=AX.X, op=ALU.max, reads=RT("lg"), writes=RT("gmax"))
                        I("dve", "tensor_tensor", out=rt["gsh"][:], in0=gl, in1=b84(rt["gmax"][:]), op=ALU.subtract, reads=RT("lg", "gmax"), writes=RT("gsh"))
                        I("dve", "tensor_scalar", out=rt["gone"][:], in0=rt["gsh"][:], scalar1=0.0, scalar2=None, op0=ALU.is_ge, reads=RT("gsh"), writes=RT("gone"))
                        I("act", "activation", out=rt["gsh"][:], in_=rt["gsh"][:], func=AF.Exp, reads=RT("gsh"), writes=RT("gsh"))
                        I("dve", "tensor_reduce", out=rt["gsum"][:], in_=rt["gsh"][:], axis=AX.X, op=ALU.add, reads=RT("gsh"), writes=RT("gsum"))
                        I("dve", "reciprocal", out=rt["gsum"][:], in_=rt["gsum"][:], reads=RT("gsum"), writes=RT("gsum"))
                        I("dve", "tensor_tensor", out=rt["t16"][:].rearrange("p q (g e) -> p q g e", g=4), in0=el,
                          in1=rt["gone"][:].unsqueeze(3).broadcast_to([128, 8, 4, 4]), op=ALU.mult, reads=RT("lg", "gone"), writes=RT("t16"))
                        I("dve", "tensor_reduce", out=rt["els"][:], in_=rt["t16"][:].rearrange("p q (g e) -> p q e g", g=4), axis=AX.X, op=ALU.add,
                          reads=RT("t16"), writes=RT("els"))
                        I("dve", "tensor_reduce", out=rt["e1"][:], in_=rt["els"][:], axis=AX.X, op=ALU.max, reads=RT("els"), writes=RT("e1"))
                        I("dve", "tensor_tensor", out=rt["m1"][:], in0=rt["els"][:], in1=b84(rt["e1"][:]), op=ALU.is_ge, reads=RT("els", "e1"), writes=RT("m1"))
                        I("dve", "scalar_tensor_tensor", out=rt["els2"][:], in0=rt["m1"][:], scalar=-1e30, in1=rt["els"][:], op0=ALU.mult, op1=ALU.add,
                          reads=RT("m1", "els"), writes=RT("els2"))
                        I("dve", "tensor_reduce", out=rt["e2"][:], in_=rt["els2"][:], axis=AX.X, op=ALU.max, reads=RT("els2"), writes=RT("e2"))
                        I("dve", "tensor_tensor", out=rt["m2"][:], in0=rt["els2"][:], in1=b84(rt["e2"][:]), op=ALU.is_ge, reads=RT("els2", "e2"), writes=RT("m2"))
                        I("dve", "tensor_tensor", out=rt["r"][:], in0=rt["e2"][:], in1=rt["e1"][:], op=ALU.subtract, reads=RT("e1", "e2"), writes=RT("r"))
                        I("act", "activation", out=rt["r"][:], in_=rt["r"][:], func=AF.Exp, reads=RT("r"), writes=RT("r"))
                        I("dve", "tensor_scalar", out=rt["w1"][:], in0=rt["r"][:], scalar1=1.0, scalar2=None, op0=ALU.add, reads=RT("r"), writes=RT("w1"))
                        I("dve", "reciprocal", out=rt["w1"][:], in_=rt["w1"][:], reads=RT("w1"), writes=RT("w1"))
                        I("dve", "tensor_tensor", out=rt["w1"][:], in0=rt["w1"][:], in1=rt["gsum"][:], op=ALU.mult, reads=RT("w1", "gsum"), writes=RT("w1"))
                        I("dve", "tensor_tensor", out=rt["w2"][:], in0=rt["w1"][:], in1=rt["r"][:], op=ALU.mult, reads=RT("w1", "r"), writes=RT("w2"))
                        I("dve", "tensor_tensor", out=rt["m1"][:], in0=rt["m1"][:], in1=b84(rt["w1"][:]), op=ALU.mult, reads=RT("m1", "w1"), writes=RT("m1"))
                        I("dve", "tensor_tensor", out=rt["m2"][:], in0=rt["m2"][:], in1=b84(rt["w2"][:]), op=ALU.mult, reads=RT("m2", "w2"), writes=RT("m2"))
                        I("dve", "tensor_tensor", out=rt["ew"][:], in0=rt["m1"][:], in1=rt["m2"][:], op=ALU.add, reads=RT("m1", "m2"), writes=RT("ew"))
                        I("dve", "tensor_tensor", out=comb[:].rearrange("p q (g e) -> p q g e", g=4),
                          in0=rt["gone"][:].unsqueeze(3).broadcast_to([128, 8, 4, 4]), in1=rt["ew"][:].unsqueeze(2).broadcast_to([128, 8, 4, 4]),
                          op=ALU.mult, reads=RT("gone", "ew"), writes=RT("comb"))

                        if debug:
                            DMA("sp", dbg["comb"], comb[:].rearrange("p a b -> p (a b)"), reads=[("comb",)], key="dbg3")
                    for fs in range(2):
                        fb = fq * 2 + fs
                        for tg in range(2):
                            ig = guc % 4
                            iu = (guc + 1) % 4
                            guc += 2
                            for c in range(NCH):
                                I("pe", "matmul", psGU[ig][:], lhsT=Wgu[sg_][:, c, fs * 128:(fs + 1) * 128], rhs=h2T[:, c, tg * 512:(tg + 1) * 512],
                                  start=(c == 0), stop=(c == NCH - 1), reads=[("Wgu", sg_), ("h2T",)], writes=[psGUn[ig]])
                            for c in range(NCH):
                                I("pe", "matmul", psGU[iu][:], lhsT=Wgu[su_][:, c, fs * 128:(fs + 1) * 128], rhs=h2T[:, c, tg * 512:(tg + 1) * 512],
                                  start=(c == 0), stop=(c == NCH - 1), reads=[("Wgu", su_), ("h2T",)], writes=[psGUn[iu]])
                            sb = sgc % 2
                            sgc += 1
                            I("act", "activation", out=sgt[sb], in_=psGU[ig][:], func=AF.Silu, reads=[psGUn[ig]], writes=[("sgt", sb)])
                            I("dve", "tensor_tensor", out=hidT[:, fb, tg * 512:(tg + 1) * 512], in0=psGU[iu][:], in1=sgt[sb], op=ALU.mult,
                              reads=[psGUn[iu], ("sgt", sb)], writes=[("hidT", fb, tg)])
                for db in range(4):
                    ds_ = dsl % 2
                    dsl += 1
                    DMA("pool", Wd[ds_][:], w_down[ex, :, db * 512:(db + 1) * 512].rearrange("(c p) n -> p c n", p=128),
                        writes=[("Wd", ds_)], key=("Wd", ds_))
                    for i in range(8):
                        iy = yc % 2
                        yc += 1
                        for fb in range(8):
                            I("pe", "matmul", psY[iy][:], lhsT=hidT[:, fb, i * 128:(i + 1) * 128], rhs=Wd[ds_][:, fb, :], start=(fb == 0), stop=(fb == 7),
                              reads=[("hidT", fb, i // 4), ("Wd", ds_)], writes=[psYn[iy]])
                        I("dve", "scalar_tensor_tensor", out=x1[:, i, db * 512:(db + 1) * 512], in0=psY[iy][:], scalar=comb[:, i, ex:ex + 1],
                          in1=x1[:, i, db * 512:(db + 1) * 512], op0=ALU.mult, op1=ALU.add,
                          reads=[psYn[iy], ("comb",), ("x1", i, db)], writes=[("x1", i, db)])
                P.maybe_barrier(900)
            for i in range(8):
                I("act", "activation", out=junk, in_=x1[:, i, :], func=AF.Square, accum_out=ss[:, 24 + i:25 + i],
                  reads=[("x1", i)], writes=[("junk",), ("ss", 24 + i)])
                I("act", "activation", out=rstd[:, 24 + i:25 + i], in_=ss[:, 24 + i:25 + i], func=AF.Sqrt, bias=epsc[:], scale=1.0 / D,
                  reads=[("ss", 24 + i), ("epsc",)], writes=[("rstd", 24 + i)])
                I("dve", "reciprocal", out=rstd[:, 24 + i:25 + i], in_=rstd[:, 24 + i:25 + i], reads=[("rstd", 24 + i)], writes=[("rstd", 24 + i)])
                I("dve", "scalar_tensor_tensor", out=x1[:, i, :], in0=x1[:, i, :], scalar=rstd[:, 24 + i:25 + i], in1=gB[:],
                  op0=ALU.mult, op1=ALU.mult, reads=[("x1", i), ("rstd", 24 + i), ("gB",)], writes=[("x1", i)])
                DMA("sp", y_out[i * 128:(i + 1) * 128, :], x1[:, i, :], reads=[("x1", i)], key=("yo", i % 2))
            P.emit()
    return nc


def _consts(half):
    bf = ml_dtypes.bfloat16
    slopes = 2.0 ** (-8.0 * np.arange(1, NH + 1) / NH)
    c = {}
    c["c_ident"] = np.eye(128, dtype=np.float32).astype(bf)
    gmk = np.zeros((3, 8, 8), np.float32)
    nmin = 0 if half == 1 else 4
    for qt in range(8):
        qb = 4 + qt // 2
        for n in range(8):
            past = (n < qb) and (n >= nmin)
            gmk[0, qt, n] = 1.0 if past else 0.0
            gmk[1, qt, n] = 1.0 if n == qb else 0.0
            gmk[2, qt, n] = 0.0 if past else -1e30
    c["c_gmask"] = np.ascontiguousarray(np.broadcast_to(gmk.reshape(1, 192), (128, 192))).astype(np.float32)
    en = np.zeros((128, 8, 128), np.float32)
    for n in range(8):
        en[n, n, :] = 1.0
        en[8, n, :] = 1.0
        en[9, n, :] = 1.0
    c["c_en"] = en.reshape(128, 1024).astype(bf)
    cb = np.zeros((128, 4, 512), np.float32)
    cc = np.arange(128)[:, None]
    tt = np.arange(512)[None, :]
    for j in range(4):
        cb[:, j, :] = np.where(tt >= 128 * j + cc, 0.0, NEG)
    c["c_cb"] = cb.reshape(128, 2048).astype(bf)
    bc = np.zeros((128, 8, 2, 16), np.float32)
    p = np.arange(128)
    for h in range(8):
        for G in range(2):
            for kt in range(16):
                bc[:, h, G, kt] = -slopes[h] * (1024 + 512 * G - (128 * kt + p))
    c["c_bcol"] = bc.reshape(128, 256).astype(np.float32)
    ar = np.zeros((8, 2, 1024), np.float32)
    trel = np.arange(1024) % 512
    for h in range(8):
        ar[h, 0] = -slopes[h] * (128 * (trel // 128))
        ar[h, 1] = -slopes[h] * (trel % 128)
    c["c_arow"] = ar.astype(bf)
    m01 = (np.arange(64)[:, None] <= np.arange(64)[None, :]).astype(np.float32)
    c["c_m01"] = m01
    return c


_NC_CACHE = {}


def kernel(x, norm_mix_g, w_in, hgrn_lb_logits, hgrn_out_norm_g, w_out, norm_ffn_g,
           w_group_router, b_group_router, w_expert_router, b_expert_router,
           w_gate, w_up, w_down, final_norm_g, _debug=False, _upto=9, _nheads=NH, _ncores=8):
    f32 = np.float32
    x = np.asarray(x, f32)
    B = x.shape[0]
    w_in0 = np.ascontiguousarray(np.asarray(w_in, f32)[0])
    w_out0 = np.ascontiguousarray(np.asarray(w_out, f32)[0])
    wg = np.ascontiguousarray(np.asarray(w_gate, f32)[0])
    wu = np.ascontiguousarray(np.asarray(w_up, f32)[0])
    wd = np.ascontiguousarray(np.asarray(w_down, f32)[0])
    wgr = np.asarray(w_group_router, f32)[0]
    wer = np.asarray(w_expert_router, f32)[0]
    wr = np.ascontiguousarray(np.concatenate([wgr] + [wer[g] for g in range(4)], axis=1))
    br = np.ascontiguousarray(np.concatenate([np.asarray(b_group_router, f32)[0].reshape(-1),
                                              np.asarray(b_expert_router, f32)[0].reshape(-1)]).reshape(1, 20))
    lbl = np.asarray(hgrn_lb_logits, f32)
    lbl_t = np.ascontiguousarray(lbl.reshape(2, 8, 128).transpose(2, 0, 1).reshape(128, 16))
    ong = np.ascontiguousarray(np.asarray(hgrn_out_norm_g, f32)[0].reshape(128, 1))
    g_mix = np.ascontiguousarray(np.asarray(norm_mix_g, f32)[0].reshape(1, D))
    g_ffn = np.ascontiguousarray(np.asarray(norm_ffn_g, f32)[0].reshape(1, D))
    g_fin = np.ascontiguousarray(np.asarray(final_norm_g, f32).reshape(1, D))

    key = (bool(_debug), _upto, _nheads)
    if key not in _NC_CACHE:
        _NC_CACHE[key] = build_nc(debug=_debug, upto=_upto, nheads=_nheads)
    nc = _NC_CACHE[key]
    consts = [_consts(0), _consts(1)]
    in_maps = []
    for core in range(_ncores):
        b, half = core // 2, core % 2
        xcore = np.zeros((TT, D), f32)
        if half == 1:
            xcore[:TO] = x[b, :TO]
            xcore[TO:] = x[b, TO:]
        else:
            xcore[TO:] = x[b, :TO]
        m = dict(xc=xcore, w_in=w_in0, w_out=w_out0, w_gate=wg, w_up=wu, w_down=wd, wr=wr, br=br,
                 g_mix=g_mix, g_ffn=g_ffn, g_fin=g_fin, lbl=lbl_t, ong=ong)
        m.update(consts[half])
        in_maps.append(m)
    res = run_bass_kernel_spmd(nc, in_maps, core_ids=list(range(_ncores)))
    out = np.zeros((B, 2048, D), f32)
    for core in range(_ncores):
        b, half = core // 2, core % 2
        out[b, half * TO:(half + 1) * TO] = res.results[core]["y"]
    if _debug:
        return out, res.results
    return out
```
